# Optimizing a Trainium2 kernel written in Bass

```python
import jax, jax.numpy as jnp
from jax import lax
import numpy as np

D_MODEL = 1024
BATCH = 16
SEQ = 4096
DEPTH = 2

CHUNK = 64
EPS = 1e-6
HA_HEADS = 4
HA_DK = 128
HA_DV = 128
HA_K = HA_HEADS * HA_DK
HA_V = HA_HEADS * HA_DV
RB_HEADS = 4
RB_DK = 128
RB_DV = 256
RB_K = RB_HEADS * RB_DK
RB_V = RB_HEADS * RB_DV
ROPE_BASE = 10000.0
IN_COLS = (HA_K, HA_K, HA_V, HA_V, RB_K, RB_K, RB_V, RB_V, D_MODEL, D_MODEL)
IN_TOTAL = sum(IN_COLS)
N_EXPERTS = 16
N_GROUPS = 4
EXP_PER_GROUP = N_EXPERTS // N_GROUPS
TOP_K = 2
D_EXPERT = 512

kernel_name = 'hybrid_hgrn2_retention_groupmoe_adaln'


def rms_norm(x, g):
    xf = x.astype(jnp.float32)
    y = xf * lax.rsqrt(jnp.mean(xf * xf, axis=-1, keepdims=True) + EPS)
    return (y * g.astype(jnp.float32)).astype(x.dtype)


def head_layer_norm(o, g):
    mu = jnp.mean(o, axis=-1, keepdims=True)
    oc = o - mu
    var = jnp.mean(oc * oc, axis=-1, keepdims=True)
    return oc * lax.rsqrt(var + EPS) * g.astype(jnp.float32)


def to_chunks(t, heads):
    b, s, w = t.shape
    return t.reshape(b, s // CHUNK, CHUNK, heads, w // heads).transpose(1, 0, 3, 2, 4)


def from_chunks(t):
    n, b, h, c, d = t.shape
    return t.transpose(1, 0, 3, 2, 4).reshape(b, n * c, h, d)


def rotary(t, heads):
    b, s, w = t.shape
    d = w // heads
    half = d // 2
    t = t.reshape(b, s, heads, d)
    inv = ROPE_BASE ** (-jnp.arange(half, dtype=jnp.float32) / half)
    ang = jnp.arange(s, dtype=jnp.float32)[:, None] * inv[None, :]
    cos = jnp.cos(ang)[None, :, None, :]
    sin = jnp.sin(ang)[None, :, None, :]
    t1, t2 = t[..., :half], t[..., half:]
    out = jnp.concatenate([t1 * cos - t2 * sin, t1 * sin + t2 * cos], axis=-1)
    return out.reshape(b, s, w)


def hgrn2_scan(q, k, v, log_f):
    n, b, h, c, dk = q.shape
    dv = v.shape[-1]
    causal = jnp.tril(jnp.ones((c, c), dtype=bool))[:, :, None]

    def step(state, xs):
        qn, kn, vn, gn = xs
        cum = jnp.cumsum(gn, axis=2)
        diff = cum[:, :, :, None, :] - cum[:, :, None, :, :]
        decay = jnp.exp(jnp.where(causal, diff, -jnp.inf))
        scores = jnp.einsum('bhjd,bhld,bhjld->bhjl', qn, kn, decay)
        out = (jnp.einsum('bhjl,bhlv->bhjv', scores, vn)
               + jnp.einsum('bhjd,bhdv->bhjv', qn * jnp.exp(cum), state))
        last = cum[:, :, -1:, :]
        state = (jnp.exp(last[:, :, 0, :, None]) * state
                 + jnp.einsum('bhld,bhlv->bhdv', kn * jnp.exp(last - cum), vn))
        return state, out

    init = jnp.zeros((b, h, dk, dv), jnp.float32)
    _, out = lax.scan(step, init, (q, k, v, log_f))
    return out


def retention_scan(q, k, v):
    n, b, h, c, dk = q.shape
    dv = v.shape[-1]
    log_gamma = jnp.log(1.0 - 2.0 ** (-5.0 - jnp.arange(h, dtype=jnp.float32)))
    pos = jnp.arange(c, dtype=jnp.float32)
    intra = jnp.exp(log_gamma[:, None, None] * jnp.abs(pos[:, None] - pos[None, :]))
    q_dec = jnp.exp(log_gamma[:, None] * (pos + 1.0))[:, :, None]
    k_dec = jnp.exp(log_gamma[:, None] * (c - 1.0 - pos))[:, :, None]
    chunk_dec = jnp.exp(log_gamma * c)[:, None, None]

    def step(state, xs):
        qn, kn, vn = xs
        scores = jnp.einsum('bhjd,bhld->bhjl', qn, kn) * intra
        out = (jnp.einsum('bhjl,bhlv->bhjv', scores, vn)
               + jnp.einsum('bhjd,bhdv->bhjv', qn, state) * q_dec)
        state = chunk_dec * state + jnp.einsum('bhld,bhlv->bhdv', kn * k_dec, vn)
        return state, out

    init = jnp.zeros((b, h, dk, dv), jnp.float32)
    _, out = lax.scan(step, init, (q, k, v))
    return out


def hybrid_mixer(h, w_in, lb, g_hgrn, g_ret, w_branch_a, w_branch_b, w_out):
    bsz, s, _ = h.shape
    f32 = jnp.float32
    z = h @ w_in
    bounds = np.cumsum(IN_COLS)[:-1].tolist()
    q_a, f_a, i_a, og_a, q_b, k_b, v_b, og_b, m_a, m_b = jnp.split(z, bounds, axis=-1)

    f = lb + (1.0 - lb) * jax.nn.sigmoid(f_a.astype(f32))
    o_a = hgrn2_scan(to_chunks(jax.nn.silu(q_a.astype(f32)), HA_HEADS),
                     to_chunks(1.0 - f, HA_HEADS),
                     to_chunks(i_a.astype(f32), HA_HEADS),
                     to_chunks(jnp.log(f), HA_HEADS))
    o_a = rms_norm(from_chunks(o_a), g_hgrn).reshape(bsz, s, HA_V)
    y_a = (o_a * jax.nn.silu(og_a.astype(f32))).astype(h.dtype) @ w_branch_a

    qr = rotary(q_b.astype(f32), RB_HEADS) * (RB_DK ** -0.5)
    kr = rotary(k_b.astype(f32), RB_HEADS)
    o_b = retention_scan(to_chunks(qr, RB_HEADS), to_chunks(kr, RB_HEADS),
                         to_chunks(v_b.astype(f32), RB_HEADS))
    o_b = head_layer_norm(from_chunks(o_b), g_ret).reshape(bsz, s, RB_V)
    y_b = (o_b * jax.nn.silu(og_b.astype(f32))).astype(h.dtype) @ w_branch_b

    y = jax.nn.sigmoid(m_a) * y_a + jax.nn.sigmoid(m_b) * y_b
    return y @ w_out


def grouped_moe(h, w_router, b_router, w_gate, w_up, w_down):
    bsz, s, d = h.shape
    t = h.reshape(-1, d)
    n_tok = t.shape[0]
    scores = jax.nn.sigmoid((t @ w_router).astype(jnp.float32))
    biased = scores + b_router.astype(jnp.float32)
    grp = biased.reshape(n_tok, N_GROUPS, EXP_PER_GROUP)
    grp_score = lax.top_k(grp, TOP_K)[0].sum(axis=-1)
    g_sel = jnp.argmax(grp_score, axis=-1)
    in_grp = grp[jnp.arange(n_tok), g_sel]
    _, local = lax.top_k(in_grp, TOP_K)
    expert_idx = g_sel[:, None] * EXP_PER_GROUP + local
    w_sel = jnp.take_along_axis(scores, expert_idx, axis=1)
    w_sel = w_sel / jnp.sum(w_sel, axis=-1, keepdims=True)
    combine = jnp.sum(jax.nn.one_hot(expert_idx, N_EXPERTS, dtype=jnp.float32) * w_sel[..., None], axis=1)
    out = jnp.zeros((n_tok, d), jnp.float32)
    for e in range(N_EXPERTS):
        he = jax.nn.silu(t @ w_gate[e]) * (t @ w_up[e])
        out = out + combine[:, e:e + 1] * (he @ w_down[e]).astype(jnp.float32)
    return out.astype(h.dtype).reshape(bsz, s, d)


def setup_inputs(seed: int = 0) -> dict:
    key = jax.random.key(seed)
    ks = jax.random.split(key, 20)
    f32 = jnp.float32
    D = D_MODEL

    def nrm(k, shape, scale):
        return jax.random.normal(k, shape, f32) * scale

    return {
        'x': nrm(ks[0], (BATCH, SEQ, D), 1.0),
        'c': nrm(ks[1], (BATCH, D), 1.0),
        'w_ada': nrm(ks[2], (DEPTH, D, 6 * D), 0.5 * D ** -0.5),
        'b_ada': nrm(ks[3], (DEPTH, 6 * D), 0.02),
        'g_norm1': 1.0 + nrm(ks[4], (DEPTH, D), 0.02),
        'g_norm2': 1.0 + nrm(ks[5], (DEPTH, D), 0.02),
        'w_in': nrm(ks[6], (DEPTH, D, IN_TOTAL), D ** -0.5),
        'lb_logits': nrm(ks[7], (DEPTH, HA_K), 0.1),
        'g_hgrn': 1.0 + nrm(ks[8], (DEPTH, HA_HEADS, HA_DV), 0.02),
        'g_ret': 1.0 + nrm(ks[9], (DEPTH, RB_HEADS, RB_DV), 0.02),
        'w_branch_a': nrm(ks[10], (DEPTH, HA_V, D), HA_V ** -0.5),
        'w_branch_b': nrm(ks[11], (DEPTH, RB_V, D), RB_V ** -0.5),
        'w_out': nrm(ks[12], (DEPTH, D, D), D ** -0.5),
        'w_router': nrm(ks[13], (D, N_EXPERTS), D ** -0.5),
        'b_router': nrm(ks[14], (N_EXPERTS,), 0.01),
        'w_exp_gate': nrm(ks[15], (DEPTH, N_EXPERTS, D, D_EXPERT), D ** -0.5),
        'w_exp_up': nrm(ks[16], (DEPTH, N_EXPERTS, D, D_EXPERT), D ** -0.5),
        'w_exp_down': nrm(ks[17], (DEPTH, N_EXPERTS, D_EXPERT, D), D_EXPERT ** -0.5),
        'g_final': 1.0 + nrm(ks[18], (D,), 0.02),
    }


def reference(x, c, w_ada, b_ada, g_norm1, g_norm2, w_in, lb_logits, g_hgrn, g_ret,
              w_branch_a, w_branch_b, w_out, w_router, b_router, w_exp_gate, w_exp_up,
              w_exp_down, g_final):
    p = jax.nn.softmax(lb_logits.astype(jnp.float32), axis=0)
    lower_bounds = jnp.cumsum(p, axis=0) - p[0:1]
    c_act = jax.nn.silu(c)
    for l in range(DEPTH):
        mod = c_act @ w_ada[l] + b_ada[l]
        sh1, sc1, gt1, sh2, sc2, gt2 = jnp.split(mod[:, None, :], 6, axis=-1)
        h = rms_norm(x, g_norm1[l]) * (1.0 + sc1) + sh1
        x = x + gt1 * hybrid_mixer(h, w_in[l], lower_bounds[l], g_hgrn[l], g_ret[l],
                                   w_branch_a[l], w_branch_b[l], w_out[l])
        h = rms_norm(x, g_norm2[l]) * (1.0 + sc2) + sh2
        x = x + gt2 * grouped_moe(h, w_router, b_router, w_exp_gate[l], w_exp_up[l], w_exp_down[l])
    return rms_norm(x, g_final)
```

```python
import numpy as np
from contextlib import ExitStack
import concourse.bass as bass
import concourse.mybir as mybir
from concourse.bass_utils import run_bass_kernel_spmd

F32 = mybir.dt.float32
BF16 = mybir.dt.bfloat16
AF = mybir.ActivationFunctionType
ALU = mybir.AluOpType
AX = mybir.AxisListType

D = 1024
TT = 512
NSUB = 4
EPS = 1e-6
NCORE = 8
NSLOT = 4


class Res:
    __slots__ = ("name", "w", "r", "dsem")

    def __init__(self, name):
        self.name = name
        self.w = None
        self.r = {}
        self.dsem = None


class V:
    __slots__ = ("ap", "res")

    def __init__(self, ap, res):
        self.ap = ap
        self.res = res

    def __getitem__(self, idx):
        return V(self.ap[idx], self.res)

    def v(self, ap):
        return V(ap, self.res)

    def re(self, s, **kw):
        return V(self.ap.rearrange(s, **kw), self.res)

    def bc(self, shape):
        return V(self.ap.to_broadcast(shape), self.res)

    def bitcast(self, dt):
        return V(self.ap.bitcast(dt), self.res)


class Sched:
    def __init__(self, nc, st):
        self.nc = nc
        self.st = st
        self.eng = {"pe": nc.tensor, "act": nc.scalar, "dve": nc.vector, "pool": nc.gpsimd, "sp": nc.sync}
        self.sems = {}
        self.cnt = {}
        self.seen = {k: {} for k in self.eng}
        for k in self.eng:
            self.sems[k] = st.enter_context(nc.semaphore("sem_" + k))
            self.cnt[k] = 0
        self.ndsem = 0
        self.nwait = 0
        self.ninst = 0

    def new_dsem(self):
        key = ("d", self.ndsem)
        self.ndsem += 1
        self.sems[key] = self.st.enter_context(self.nc.semaphore("dsem%d" % key[1]))
        self.cnt[key] = 0
        return key

    def _deps(self, reads, writes, skipkey=None):
        deps = {}

        def add(tok):
            if tok is None:
                return
            k, v = tok
            if k == skipkey:
                return
            if deps.get(k, 0) < v:
                deps[k] = v
        for r in reads:
            add(r.w)
        for w in writes:
            add(w.w)
            for k, v in w.r.items():
                add((k, v))
        return deps

    def _wait(self, E, deps):
        seen = self.seen[E]
        for k, v in deps.items():
            if seen.get(k, 0) >= v:
                continue
            self.eng[E].wait_ge(self.sems[k], v)
            seen[k] = v
            self.nwait += 1

    def _mark(self, tok, reads, writes):
        k, v = tok
        for r in reads:
            if r.r.get(k, 0) < v:
                r.r[k] = v
        for w in writes:
            w.w = tok
            w.r = {}

    def op(self, E, fn, reads, writes, inc=True):
        deps = self._deps(reads, writes)
        if E == "pe":
            deps.pop("pe", None)
        self._wait(E, deps)
        inst = fn()
        self.ninst += 1
        if inc:
            self.cnt[E] += 1
            inst.then_inc(self.sems[E], 1)
            tok = (E, self.cnt[E])
        else:
            tok = (E, self.cnt[E] + 1)
        self._mark(tok, reads, writes)
        return inst

    def dma(self, Q, out, in_, **kw):
        reads, writes = in_.res, out.res
        wres = writes[0]
        if wres.dsem is None:
            wres.dsem = self.new_dsem()
        key = wres.dsem
        self._wait(Q, self._deps(reads, writes, skipkey=key))
        inst = self.eng[Q].dma_start(out=out.ap, in_=in_.ap, **kw)
        self.ninst += 1
        self.cnt[key] += 16
        inst.then_inc(self.sems[key], 16)
        self._mark((key, self.cnt[key]), reads, writes)


def _res(*vs):
    out = []
    for v in vs:
        if isinstance(v, V):
            for r in v.res:
                if r not in out:
                    out.append(r)
    return out


def _ap(v):
    return v.ap if isinstance(v, V) else v


def build(S=4096, dbg=None):
    NT = S // TT
    nc = bass.Bass("TRN2", target_bir_lowering=False)
    st = ExitStack()
    with st:
        K = Sched(nc, st)

        def din(name, shape, dt=F32):
            return V(nc.dram_tensor(name, list(shape), dt, kind="ExternalInput").ap(), [Res(name)])

        x_d = din("x", [2, S, D])
        cT_d = din("cT", [128, 8, 2])
        w_ada_d = din("w_ada", [2, D, 6 * D])
        b_adaT_d = din("b_adaT", [128, 2, 48])
        g1T_d = din("g1T", [128, 2, 8])
        g2T_d = din("g2T", [128, 2, 8])
        w_in_d = din("w_in", [2, D, 7168])
        lbT_d = din("lbT", [128, 2, 4])
        ghT_d = din("ghT", [128, 2, 4])
        grT_d = din("grT", [128, 2, 8])
        wa_d = din("w_branch_a", [2, 512, D])
        wb_d = din("w_branch_b", [2, D, D])
        wo_d = din("w_out", [2, D, D])
        wr_d = din("w_routerT", [128, 8, 16])
        br_d = din("b_router_bc", [128, 16])
        wg_d = din("w_exp_gate", [2, 16, D, 512])
        wu_d = din("w_exp_up", [2, 16, D, 512])
        wd_d = din("w_exp_down", [2, 16, 512, D])
        gfin_d = din("g_final_bc", [128, D])
        ident_d = din("c_ident", [128, 128])
        prot_d = din("c_prot", [128, 128])
        maskA_d = din("c_maskA", [128, 512])
        intra_d = din("c_intra", [128, 512])
        qdec_d = din("c_qdec", [128, 512])
        kdec_d = din("c_kdec", [128, 512])
        smask_d = din("c_smask", [128, 512])
        cos_d = din("c_cos", [128, S])
        sin_d = din("c_sin", [128, S])
        out_ap = nc.dram_tensor("out", [2, S, D], F32, kind="ExternalOutput").ap()
        out_sub = [V(out_ap, [Res("out%d" % s_)]) for s_ in range(NSUB)]
        scr_d = V(nc.dram_tensor("mod_scr", [2, 48, 2, 128], F32, kind="Internal").ap(), [Res("scr")])
        if dbg is not None:
            dbg_d = V(nc.dram_tensor("dbg", list(dbg), F32, kind="ExternalOutput").ap(), [Res("dbg")])

        def sb(name, shape, dt):
            t = st.enter_context(nc.sbuf_tensor("s_" + name, list(shape), dt))
            return V(t[:], [Res(name)])

        xt = [sb("x%d" % s, [128, D], F32) for s in range(NSUB)]
        hT = sb("hT", [128, 8, TT], BF16)
        ring = [sb("ring%d" % i, [128, 4096], BF16) for i in range(NSLOT)]
        BP = [sb("bp%d" % i, [128, 4, TT], BF16) for i in range(11)]
        Fe = sb("Fe", [128, 4, TT], F32)
        FBt = st.enter_context(nc.sbuf_tensor("s_FB", [128, 6, TT], F32))
        FBres = [Res("FB%d" % i) for i in range(6)]
        FBv = [V(FBt[:, i, :], [FBres[i]]) for i in range(6)]
        F1, F2, F3 = FBv[0:2], FBv[2:4], FBv[4:6]
        GB = [V(FBt[:, 2 * i:2 * i + 2, :].bitcast(BF16).rearrange("p a (k c) -> p (a k) c", k=2), [FBres[2 * i], FBres[2 * i + 1]])
              for i in range(3)]
        kinv_s = [sb("kinv_s%d" % i, [128, 512], BF16) for i in range(2)]
        ATa_s = [sb("ATa_s%d" % i, [128, 512], BF16) for i in range(2)]
        krd_s = [sb("krd_s%d" % i, [128, 512], BF16) for i in range(2)]
        ATb_s = [sb("ATb_s%d" % i, [128, 512], BF16) for i in range(2)]
        xb4 = [sb("xbn%d" % i, [128, D], BF16) for i in range(4)]
        xb = xb4
        tmpf = [sb("tmpf%d" % i, [128, TT], F32) for i in range(4)]
        tmpb = [sb("tmpb%d" % i, [128, TT], BF16) for i in range(3)]
        S_a = [[sb("S_a%d_%d" % (l, i), [128, 512], F32) for i in range(2)] for l in range(2)]
        R_b = [sb("R_b%d" % l, [128, 1024], F32) for l in range(2)]
        S_bf = [sb("S_bf%d" % i, [128, 512], BF16) for i in range(4)]
        R_bf = [sb("R_bf%d" % i, [128, 1024], BF16) for i in range(3)]
        stmp = sb("stmp", [128, 512], F32)
        gtb = sb("gtb", [128, D], F32)
        gfin = sb("gfin", [128, D], F32)
        cosS = sb("cosS", [128, TT], F32)
        sinS = sb("sinS", [128, TT], F32)
        ident = sb("ident", [128, 128], BF16)
        prot = sb("prot", [128, 128], BF16)
        ones = sb("ones", [128, 128], BF16)
        maskA = sb("maskA", [128, 512], F32)
        intra = sb("intra", [128, 512], F32)
        qdec = sb("qdec", [128, 512], F32)
        kdec = sb("kdec", [128, 512], F32)
        smask = sb("smask", [128, 512], F32)
        cT = sb("cT", [128, 8, 2], F32)
        ca = sb("ca", [128, 8, 2], F32)
        b_adaT = sb("b_adaT", [128, 2, 48], F32)
        modT = sb("modT", [128, 2, 48, 2], F32)
        modrow = sb("modrow", [96, 128], F32)
        g1T = sb("g1T", [128, 2, 8], F32)
        g2T = sb("g2T", [128, 2, 8], F32)
        A1T = sb("A1T", [128, 2, 2, 8], F32)
        A2T = sb("A2T", [128, 2, 2, 8], F32)
        lbT = sb("lbT", [128, 2, 4], F32)
        oml = sb("oml", [128, 2, 4], F32)
        ghT = sb("ghT", [128, 2, 4], F32)
        grT = sb("grT", [128, 2, 8], F32)
        wr = sb("wr", [128, 8, 16], BF16)
        br = sb("br", [128, 16], F32)
        ss = sb("ss", [128, 8], F32)
        rstd = sb("rstd", [128, 8], F32)
        comb = sb("comb", [128, NSUB, 16], F32)
        rt4 = [sb("rt%d" % i, [128, 64], F32) for i in range(4)]
        rs4 = [sb("rs%d" % i, [128, 16], F32) for i in range(10)]
        rs = rs4

        if dbg is not None:
            tapbuf = sb("tapbuf", [128, 512], F32)
        pst = st.enter_context(nc.psum_tensor("ps", [128, 8, 512], F32))
        psres = [Res("ps%d" % i) for i in range(8)]
        pscur = [0]

        def bank(n=1):
            i = pscur[0]
            if n == 2 and i % 2 == 1:
                i = (i + 1) % 8
            pscur[0] = (i + n) % 8
            if n == 1:
                return V(pst[:, i, :], [psres[i]])
            return V(pst[:, i:i + 2, :], [psres[i], psres[i + 1]])

        def mm(out, lhsT, rhs, start=True, stop=True, last=None, tp=None):
            if last is None:
                last = stop
            kw = {}
            if tp is not None:
                kw["tile_position"] = tp
            K.op("pe", lambda: nc.tensor.matmul(out.ap, lhsT.ap, rhs.ap, start=start, stop=stop, **kw),
                 _res(lhsT, rhs), _res(out), inc=last)

        def tr(out, in_, idv, last=True):
            K.op("pe", lambda: nc.tensor.transpose(out.ap, in_.ap, idv.ap), _res(in_, idv), _res(out), inc=last)

        def act(out, in_, func, scale=None, bias=None, accum=None):
            kw = {}
            if scale is not None:
                kw["scale"] = _ap(scale)
            if bias is not None:
                kw["bias"] = _ap(bias)
            if accum is not None:
                kw["accum_out"] = accum.ap
            K.op("act", lambda: nc.scalar.activation(out=out.ap, in_=in_.ap, func=func, **kw),
                 _res(in_, scale, bias), _res(out, accum))

        def tt(E, out, in0, in1, op):
            K.op(E, lambda: K.eng[E].tensor_tensor(out=out.ap, in0=in0.ap, in1=in1.ap, op=op),
                 _res(in0, in1), _res(out))

        def ts(E, out, in0, s1, op0, s2=None, op1=None):
            if E == "pool" and op1 is None:
                s2, op1 = 0.0, ALU.add

            def f():
                if op1 is None:
                    return K.eng[E].tensor_scalar(out=out.ap, in0=in0.ap, scalar1=_ap(s1), scalar2=None, op0=op0)
                return K.eng[E].tensor_scalar(out=out.ap, in0=in0.ap, scalar1=_ap(s1), scalar2=_ap(s2),
                                              op0=op0, op1=op1)
            K.op(E, f, _res(in0, s1, s2), _res(out))

        def stt(out, in0, scalar, in1, op0, op1):
            K.op("dve", lambda: nc.vector.scalar_tensor_tensor(out=out.ap, in0=in0.ap, scalar=_ap(scalar),
                                                                in1=in1.ap, op0=op0, op1=op1),
                 _res(in0, scalar, in1), _res(out))

        def cp(E, out, in_):
            if E == "act":
                K.op("act", lambda: nc.scalar.copy(out=out.ap, in_=in_.ap), _res(in_), _res(out))
            else:
                K.op(E, lambda: K.eng[E].tensor_copy(out=out.ap, in_=in_.ap), _res(in_), _res(out))

        def memset(E, out, val):
            K.op(E, lambda: K.eng[E].memset(out.ap, val), [], _res(out))

        ident_f = Fe[:, 0, 0:128]
        prot_f = Fe[:, 1, 0:128]
        wr_f = Fe[:, 2, 0:128].re("p (k e) -> p k e", k=8)
        epsb_t = sb("epsb", [128, 1], F32)
        memset("dve", epsb_t, EPS)
        epsb = epsb_t
        for dst, src in ((ident_f, ident_d), (prot_f, prot_d), (maskA, maskA_d), (intra, intra_d), (qdec, qdec_d),
                         (kdec, kdec_d), (smask, smask_d), (cT, cT_d), (b_adaT, b_adaT_d), (g1T, g1T_d),
                         (g2T, g2T_d), (lbT, lbT_d), (ghT, ghT_d), (grT, grT_d), (wr_f, wr_d), (br, br_d),
                         (gfin, gfin_d)):
            K.dma("sp", dst, src)
        cp("dve", ident, ident_f)
        cp("dve", prot, prot_f)
        cp("dve", wr, wr_f)
        memset("dve", ones, 1.0)
        memset("dve", oml[:, 0, :], 1.0)
        tt("dve", rs[0][:, 0:4], lbT[:, 0, :], lbT[:, 1, :], ALU.subtract)
        act(oml[:, 1, :], rs[0][:, 0:4], AF.Sigmoid)
        act(ca, cT, AF.Silu)

        sched = []

        def wslice(src_rows_ap, kb, cols):
            sched.append((src_rows_ap, kb, cols))

        ZC = dict(qa=0, fa=512, ia=1024, oga=1536, qb=2048, kb=2560, vb=3072, ogb=4096, ma=5120, mb=6144)
        for b in range(2):
            for t in range(NT):
                for l in range(2):
                    def wi(c0, l=l):
                        wslice(w_in_d[l][:, c0:c0 + 512].re("(k p) c -> p k c", p=128), 8, 512)
                    for c0 in (ZC["qa"], ZC["fa"], ZC["ia"]):
                        wi(c0)
                    for c0 in (ZC["qb"], ZC["kb"], ZC["vb"], ZC["vb"] + 512):
                        wi(c0)
                    for c0 in (ZC["ma"], ZC["ma"] + 512, ZC["mb"]):
                        wi(c0)
                    for c0 in (ZC["oga"], ZC["ogb"], ZC["ogb"] + 512):
                        wi(c0)
                    wi(ZC["mb"] + 512)
                    wslice(wa_d[l].re("(k p) c -> p k c", p=128), 4, 1024)
                    wslice(wb_d[l][:, 0:512].re("(k p) c -> p k c", p=128), 8, 512)
                    wslice(wb_d[l][:, 512:1024].re("(k p) c -> p k c", p=128), 8, 512)
                    wslice(wo_d[l][:, 0:512].re("(k p) c -> p k c", p=128), 8, 512)
                    wslice(wo_d[l][:, 512:1024].re("(k p) c -> p k c", p=128), 8, 512)
                    wslice(wg_d[l, 0].re("(k p) c -> p k c", p=128), 8, 512)
                    wslice(wu_d[l, 0].re("(k p) c -> p k c", p=128), 8, 512)
                    for e in range(16):
                        if e < 15:
                            wslice(wg_d[l, e + 1].re("(k p) c -> p k c", p=128), 8, 512)
                            wslice(wu_d[l, e + 1].re("(k p) c -> p k c", p=128), 8, 512)
                        wslice(wd_d[l, e].re("(k p) c -> p k c", p=128), 4, 1024)
        wpos = [0, 0]

        NSL = len(sched) // (2 * NT * 2)
        assert NSL * 2 * NT * 2 == len(sched)
        wscr_ap = nc.dram_tensor("w_scr", [2 * NSL, 128, 4096], BF16, kind="Internal").ap()
        scrw_res = [Res("scrw%d" % i) for i in range(NSLOT)]

        def wgetn(n):
            i = wpos[0]
            assert n <= NSLOT
            wpos[0] += n
            while wpos[1] < len(sched) and wpos[1] <= i + NSLOT - 1:
                j = wpos[1]
                src, kb, cols = sched[j]
                tl, k = divmod(j, NSL)
                lyr = tl % 2
                slot = ring[j % NSLOT]
                if tl < 2:
                    K.dma("pool", slot.re("p (k c) -> p k c", k=kb), src)
                    K.dma("sp", V(wscr_ap[lyr * NSL + k], [scrw_res[j % NSLOT]]), slot)
                else:
                    j0 = lyr * NSL + k
                    K.dma("sp", slot, V(wscr_ap[lyr * NSL + k], [scrw_res[j0 % NSLOT]]))
                wpos[1] += 1
            return [ring[j % NSLOT].re("p (k c) -> p k c", k=sched[j][1]) for j in range(i, i + n)]

        def wget():
            return wgetn(1)[0]

        adaring = [ring[i].bitcast(F32).re("p (k c) -> p k c", k=8) for i in range(2)]
        for l in range(2):
            pb = bank()
            for s in range(24):
                wsl = adaring[s % 2]
                K.dma("sp", wsl, w_ada_d[l][:, s * 256:(s + 1) * 256].re("(k p) c -> p k c", p=128))
                for cbk in range(2):
                    blk = s * 2 + cbk
                    for kb in range(8):
                        mm(pb[:, blk * 2:blk * 2 + 2], wsl[:, kb, cbk * 128:(cbk + 1) * 128], ca[:, kb, :],
                           start=(kb == 0), stop=(kb == 7))
            tt("dve", modT[:, l], pb[:, 0:96].re("p (k b) -> p k b", b=2),
               b_adaT[:, l, :].re("p (k o) -> p k o", o=1).bc([128, 48, 2]), ALU.add)
            for b in range(2):
                stt(A1T[:, l, b, :], modT[:, l, 8:16, b], 1.0, g1T[:, l, :], ALU.add, ALU.mult)
                stt(A2T[:, l, b, :], modT[:, l, 32:40, b], 1.0, g2T[:, l, :], ALU.add, ALU.mult)
            pb2 = bank()
            tr(pb2[:96, :128], modT[:, l].re("p k b -> p (k b)"), ident_f)
            cp("dve", modrow, pb2[:96, :128])
            K.dma("sp", scr_d[l].re("k b p -> (k b) p"), modrow)

        def load_gate(l, b, blk0):
            src = scr_d[l, blk0:blk0 + 8, b, :]
            K.dma("sp", gtb.re("p (k c) -> p k c", k=8), V(src.ap.partition_broadcast(128), src.res))

        def norm_p1(s, phase=None):
            if phase in (None, 0):
                act(xb4[s], xt[s], AF.Square, accum=ss[:, s:s + 1])
            if phase in (None, 1):
                ts("dve", rstd[:, s:s + 1], ss[:, s:s + 1], 1.0 / D, ALU.mult, EPS, ALU.add)
                act(rstd[:, s:s + 1], rstd[:, s:s + 1], AF.Ln)
                act(rstd[:, s:s + 1], rstd[:, s:s + 1], AF.Exp, scale=-0.5)
            if phase in (None, 2):
                ts("pool", xb4[s], xt[s], rstd[:, s:s + 1], ALU.mult)

        def norm_p2(AT, l, b, shblk, s, on_act):
            pb = bank().bitcast(BF16)
            for kb in range(8):
                tr(pb[:, kb * 128:(kb + 1) * 128], xb4[s][:, kb * 128:(kb + 1) * 128], ident, last=(kb == 7))
            if on_act:
                for kb in range(8):
                    act(hT[:, kb, s * 128:(s + 1) * 128], pb[:, kb * 128:(kb + 1) * 128], AF.Identity,
                        scale=AT[:, l, b, kb:kb + 1], bias=modT[:, l, shblk + kb, b:b + 1])
            else:
                for hf in range(2):
                    hv = tmpf[hf].re("p (k j) -> p k j", k=4)
                    tt("dve", hv, pb[:, hf * 512:(hf + 1) * 512].re("p (k j) -> p k j", k=4),
                       AT[:, l, b, 4 * hf:4 * hf + 4].re("p (k o) -> p k o", o=1).bc([128, 4, 128]), ALU.mult)
                    tt("dve", hT[:, 4 * hf:4 * hf + 4, s * 128:(s + 1) * 128], hv,
                       modT[:, l, shblk + 4 * hf:shblk + 4 * hf + 4, b:b + 1].bc([128, 4, 128]), ALU.add)

        def norm_to_hT(AT, l, b, shblk, p1_done=False, act_subs=(0,)):
            if not p1_done:
                for s in range(NSUB):
                    norm_p1(s)
            for s in range(NSUB):
                norm_p2(AT, l, b, shblk, s, s in act_subs)

        def proj_fm(W, nblk, evac):
            for blk in range(nblk):
                pb = bank()
                for kb in range(8):
                    mm(pb, W[:, kb, blk * 128:(blk + 1) * 128], hT[:, kb, :], start=(kb == 0), stop=(kb == 7))
                evac(blk, pb)

        def proj_tm(W, evac):
            for s in range(NSUB):
                pb = bank()
                for kb in range(8):
                    mm(pb, hT[:, kb, s * 128:(s + 1) * 128], W[:, kb, :], start=(kb == 0), stop=(kb == 7))
                evac(s, pb)

        tapidx = [0]
        tapnames = []

        def tap(name, v, cond=True):
            if dbg is None or not cond:
                return
            i = tapidx[0]
            if i >= dbg[0]:
                return
            tapidx[0] += 1
            tapnames.append(name)
            n = v.ap.shape[-1] if len(v.ap.shape) == 2 else None
            if v.ap.dtype != F32:
                cp("dve", tapbuf[:, 0:n], v)
                v = tapbuf[:, 0:n]
            K.dma("sp", dbg_d[i, 0:v.ap.shape[0], 0:n], v)
        build.tapnames = tapnames

        def mixer(l, b, t, prenormed, tail_cb):
            tok0 = t * TT
            T0 = (l == 0 and b == 0 and t == 0)
            K.dma("sp", cosS, cos_d[:, tok0:tok0 + TT])
            K.dma("sp", sinS, sin_d[:, tok0:tok0 + TT])
            if not prenormed:
                norm_to_hT(A1T, l, b, 0)
            qeT, kinvT, va_tm, oaT, qrT, krT, qdT = BP[0:7]
            vb_tm = [BP[7], BP[8]]
            obT = [BP[9], BP[10]]
            qsT = BP[9]
            W = wget()
            proj_fm(W, 4, lambda blk, pb: act(qsT[:, blk, :], pb, AF.Silu))
            W = wget()

            pending = []

            def pump(n):
                for _ in range(n):
                    if not pending:
                        return
                    g = pending[0]
                    try:
                        next(g)
                    except StopIteration:
                        pending.pop(0)

            def chain_f(blk, f1, f2, f3):
                act(f2, f1, AF.Ln, bias=1.0)
                yield
                act(f2, f2, AF.Exp, scale=-1.0)
                ts("dve", f1, f2, oml[:, l, blk:blk + 1], ALU.mult)
                yield
                act(f2, f1, AF.Ln, scale=-1.0, bias=1.0)
                K.op("dve", lambda: nc.vector.tensor_tensor_scan(out=f3.ap, data0=smask.ap, data1=f2.ap,
                                                                  initial=0.0, op0=ALU.mult, op1=ALU.add),
                     _res(smask, f2), _res(f3))
                yield
                act(Fe[:, blk, :], f3, AF.Exp)
                yield
                act(f2, f3, AF.Exp, scale=-1.0)
                tt("dve", kinvT[:, blk, :], f1, f2, ALU.mult)
                tt("dve", qeT[:, blk, :], qsT[:, blk, :], Fe[:, blk, :], ALU.mult)

            def ev_f(blk, pb):
                f1, f2, f3 = F1[blk % 2], F2[blk % 2], F3[blk % 2]
                while len(pending) >= 2:
                    pump(1)
                act(f1, pb, AF.Exp)
                pending.append(chain_f(blk, f1, f2, f3))
                pump(1)
            proj_fm(W, 4, ev_f)
            W = wget()

            def ev_ia(s, pb):
                cp("act", va_tm[:, s, :], pb)
                pump(2)
            proj_tm(W, ev_ia)
            rot_defer = []
            for which in range(2):
                W = wget()

                def rot_rest(blk, xbf, which):
                    pr_ = bank()
                    mm(pr_, prot, xbf)
                    ta, tb = tmpf[2 * (blk % 2)], tmpf[2 * (blk % 2) + 1]
                    tt("dve", ta, xbf, cosS, ALU.mult)
                    tt("dve", tb, pr_, sinS, ALU.mult)
                    dst = qrT if which == 0 else krT
                    tt("pool", dst[:, blk, :], ta, tb, ALU.add)
                    if which == 0:
                        tt("dve", qdT[:, blk, :].re("p (r j) -> p r j", r=4), dst[:, blk, :].re("p (r j) -> p r j", r=4),
                           qdec[:, blk * 128:(blk + 1) * 128].re("p (o j) -> p o j", o=1).bc([128, 4, 128]), ALU.mult)

                def ev_rot(blk, pb, which=which):
                    xbf = tmpb[1 + (blk % 2)]
                    if rot_defer:
                        rot_defer.pop(0)()
                    cp("act", xbf, pb)
                    rot_defer.append(lambda blk=blk, xbf=xbf, which=which: rot_rest(blk, xbf, which))
                    pump(2)
                proj_fm(W, 4, ev_rot)
            for half in range(2):
                W = wget()

                def ev_vb(s, pb, half=half):
                    while rot_defer:
                        rot_defer.pop(0)()
                    cp("act", vb_tm[half][:, s, :], pb)
                    pump(2)
                proj_tm(W, ev_vb)
            pump(1000)
            assert not pending
            tap("qeT_0", qeT[:, 0, :], T0)

            def vbv(s, h, vb, rows=slice(0, 128)):
                c0 = h * 256 + vb * 128
                return vb_tm[c0 // 512][rows, s, (c0 % 512):(c0 % 512) + 128]

            gam64 = [float((1.0 - 2.0 ** (-5.0 - h)) ** 64) for h in range(4)]
            def fill_gen():
                for dst in GB:
                    Wf = wget()
                    for blk in range(4):
                        pf = bank()
                        for kb in range(8):
                            mm(pf, Wf[:, kb, blk * 128:(blk + 1) * 128], hT[:, kb, :], start=(kb == 0), stop=(kb == 7))
                        cp("act", dst[:, blk, :], pf)
                        yield
            fg = fill_gen()

            def fill():
                next(fg, None)
            for s in range(NSUB):
                tk = slice(s * 128, (s + 1) * 128)
                kinv_tm, ATa, krd_tm, ATb = kinv_s[s % 2], ATa_s[s % 2], krd_s[s % 2], ATb_s[s % 2]
                pb = bank().bitcast(BF16)
                for h in range(4):
                    tr(pb[:, h * 128:(h + 1) * 128], kinvT[:, h, tk], ident, last=(h == 3))
                cp("act", kinv_tm, pb[:, 0:512])
                pb = bank()
                for h in range(4):
                    mm(pb[:, h * 128:(h + 1) * 128], kinvT[:, h, tk], qeT[:, h, tk], last=(h == 3))
                tt("dve", ATa, pb, maskA, ALU.mult)
                pb = bank()
                for h in range(4):
                    mm(pb[:, h * 128:(h + 1) * 128], krT[:, h, tk], qrT[:, h, tk], last=(h == 3))
                tt("dve", ATb, pb, intra, ALU.mult)
                pb = bank().bitcast(BF16)
                for h in range(4):
                    tr(pb[:, h * 128:(h + 1) * 128], krT[:, h, tk], ident, last=(h == 3))
                tt("dve", krd_tm, pb[:, 0:512], kdec, ALU.mult)
                fill()
                sbfs = []
                for cc in range(4):
                    c = s * 4 + cc
                    sbf = S_bf[c % 4]
                    Scur, Snxt = S_a[l][c % 2], S_a[l][(c + 1) % 2]
                    cp("act", sbf, Scur)
                    sbfs.append(sbf)
                    pr = slice(32 * cc, 32 * cc + 32)
                    pb = bank()
                    for h in range(4):
                        hs = slice(h * 128, (h + 1) * 128)
                        mm(pb[:, hs], kinv_tm[pr, hs], va_tm[pr, s, hs], last=(h == 3), tp=(32 * cc, 0))
                    tt("dve", stmp, Scur, pb, ALU.add)
                    col = s * 128 + 32 * cc + 31
                    tt("dve", Snxt.re("p (h v) -> p h v", h=4), stmp.re("p (h v) -> p h v", h=4),
                       Fe[:, :, col:col + 1].bc([128, 4, 128]), ALU.mult)
                rbfs = []
                for cc in range(2):
                    c = s * 2 + cc
                    rbf = R_bf[c % 3]
                    cp("act", rbf, R_b[l])
                    rbfs.append(rbf)
                    pr = slice(64 * cc, 64 * cc + 64)
                    pu = bank(2)
                    for h in range(4):
                        c0 = h * 256
                        mm(pu[:, h // 2, (h % 2) * 256:(h % 2) * 256 + 256], krd_tm[pr, h * 128:(h + 1) * 128],
                           vb_tm[c0 // 512][pr, s, (c0 % 512):(c0 % 512) + 256], last=(h == 3), tp=(64 * cc, 0))
                    for h in range(4):
                        stt(R_b[l][:, h * 256:(h + 1) * 256], R_b[l][:, h * 256:(h + 1) * 256], gam64[h],
                            pu[:, h // 2, (h % 2) * 256:(h % 2) * 256 + 256], ALU.mult, ALU.add)
                fill()
                po = bank()
                for h in range(4):
                    hs = slice(h * 128, (h + 1) * 128)
                    mm(po[:, hs], va_tm[:, s, hs], ATa[:, hs], start=True, stop=False)
                    for cc in range(4):
                        o0 = h * 128 + 32 * cc
                        mm(po[:, o0:o0 + 32], sbfs[cc][:, hs], qeT[:, h, s * 128 + 32 * cc:s * 128 + 32 * cc + 32],
                           start=False, stop=(cc == 3), last=(cc == 3 and h == 3))
                act(tmpb[0], po, AF.Square)
                po2 = bank(2)
                for h in range(4):
                    for vb in range(2):
                        o0 = ((h % 2) * 2 + vb) * 128
                        mm(po2[:, h // 2, o0:o0 + 128], vbv(s, h, vb), ATb[:, h * 128:(h + 1) * 128],
                           start=True, stop=False)
                        for cc in range(2):
                            c0 = h * 256 + vb * 128
                            mm(po2[:, h // 2, o0 + 64 * cc:o0 + 64 * cc + 64], rbfs[cc][:, c0:c0 + 128],
                               qdT[:, h, s * 128 + 64 * cc:s * 128 + 64 * cc + 64], start=False, stop=(cc == 1),
                               last=(cc == 1 and vb == 1 and h == 3))
                ob16 = [xb[0][:, 0:512], xb[0][:, 512:1024]]
                sq = [xb[1][:, 0:512], xb[1][:, 512:1024]]
                for k in range(2):
                    cp("act", ob16[k], po2[:, k, :])
                    act(sq[k], po2[:, k, :], AF.Square)
                fill()
                pn = bank()
                mm(pn, ones, tmpb[0])
                p1 = bank()
                p2 = bank()
                for h in range(4):
                    k, hh = h // 2, h % 2
                    for vb in range(2):
                        o0 = (hh * 2 + vb) * 128
                        mm(p1[:, h * 128:(h + 1) * 128], ones, ob16[k][:, o0:o0 + 128], start=(vb == 0),
                           stop=(vb == 1), last=False)
                for h in range(4):
                    k, hh = h // 2, h % 2
                    for vb in range(2):
                        o0 = (hh * 2 + vb) * 128
                        mm(p2[:, h * 128:(h + 1) * 128], ones, sq[k][:, o0:o0 + 128], start=(vb == 0),
                           stop=(vb == 1), last=(vb == 1 and h == 3))
                rs_a, mean, var = tmpf[0], tmpf[1], tmpf[2]
                act(rs_a, pn, AF.Ln, scale=1.0 / 128, bias=epsb)
                act(mean, p1, AF.Copy, scale=1.0 / 256)
                act(var, p1, AF.Square, scale=1.0 / 256)
                stt(var, p2, 1.0 / 256, var, ALU.mult, ALU.subtract)
                act(var, var, AF.Ln, bias=epsb)
                act(rs_a, rs_a, AF.Exp, scale=-0.5)
                act(var, var, AF.Exp, scale=-0.5)
                tt("dve", oaT[:, :, tk], po.re("p (h j) -> p h j", h=4), rs_a.re("p (h j) -> p h j", h=4), ALU.mult)
                for k in range(2):
                    ta = tmpf[3]
                    mv = mean[:, k * 256:(k + 1) * 256].re("p (h o j) -> p h o j", h=2, o=1).bc([128, 2, 2, 128])
                    rv = var[:, k * 256:(k + 1) * 256].re("p (h o j) -> p h o j", h=2, o=1).bc([128, 2, 2, 128])
                    tt("dve", ta.re("p (h v j) -> p h v j", h=2, v=2), po2[:, k, :].re("p (h v j) -> p h v j", h=2, v=2),
                       mv, ALU.subtract)
                    tt("pool", obT[k][:, :, tk].re("p (h v) j -> p h v j", h=2), ta.re("p (h v j) -> p h v j", h=2, v=2),
                       rv, ALU.mult)
            for _ in range(12):
                fill()
            W = wget()

            def ev_oga(blk, pb):
                sg = tmpb[1 + blk % 2]
                act(sg, pb, AF.Silu)
                stt(oaT[:, blk, :], sg, ghT[:, l, blk:blk + 1], oaT[:, blk, :], ALU.mult, ALU.mult)
            proj_fm(W, 4, ev_oga)
            for half in range(2):
                W = wget()

                def ev_ogb(blk, pb, half=half):
                    sg = tmpb[1 + blk % 2]
                    act(sg, pb, AF.Silu)
                    stt(obT[half][:, blk, :], sg, grT[:, l, half * 4 + blk:half * 4 + blk + 1], obT[half][:, blk, :],
                        ALU.mult, ALU.mult)
                proj_fm(W, 4, ev_ogb)
            tap("oaT_0", oaT[:, 0, :], T0)
            tap("oaT_3", oaT[:, 3, :], T0)
            tap("qrT_0", qrT[:, 0, :], T0)
            tap("krT_1", krT[:, 1, :], T0)
            tap("obT_0", obT[0][:, 0, :], T0)
            tap("obT_7", obT[1][:, 3, :], T0)
            yT = [BP[5], BP[6]]
            W = wget()
            proj_fm(W, 4, lambda blk, pb: cp("act", BP[4][:, blk, :], pb))
            graw_a = [GB[0], GB[1]]
            graw_b = [GB[2], BP[4]]
            Wa, Wb0_, Wb1_ = wgetn(3)
            Wb = [Wb0_, Wb1_]
            for cb in range(8):
                pa = bank()
                for vb in range(4):
                    mm(pa, Wa[:, vb, cb * 128:(cb + 1) * 128], oaT[:, vb, :], start=(vb == 0), stop=(vb == 3))
                pb = bank()
                for kb in range(8):
                    mm(pb, Wb[cb // 4][:, kb, (cb % 4) * 128:(cb % 4) * 128 + 128], obT[kb // 4][:, kb % 4, :],
                       start=(kb == 0), stop=(kb == 7))
                act(tmpb[1], graw_a[cb // 4][:, cb % 4, :], AF.Sigmoid)
                act(tmpb[2], graw_b[cb // 4][:, cb % 4, :], AF.Sigmoid)
                tt("dve", tmpf[0], pa, tmpb[1], ALU.mult)
                tt("dve", tmpf[1], pb, tmpb[2], ALU.mult)
                tt("pool", yT[cb // 4][:, cb % 4, :], tmpf[0], tmpf[1], ALU.add)
            tap("yT_0", yT[0][:, 0, :], T0)
            tap("yT_7", yT[1][:, 3, :], T0)
            load_gate(l, b, 16)
            tap("gt1", gtb, T0)
            Wo = wgetn(2)
            for s in range(NSUB):
                for half in range(2):
                    pb = bank()
                    for kb in range(8):
                        mm(pb, yT[kb // 4][:, kb % 4, s * 128:(s + 1) * 128], Wo[half][:, kb, :],
                           start=(kb == 0), stop=(kb == 7))
                    hs = slice(half * 512, (half + 1) * 512)
                    tt("dve", tmpf[2 + half], pb, gtb[:, hs], ALU.mult)
                    tt("pool" if half == 0 else "dve", xt[s][:, hs], xt[s][:, hs], tmpf[2 + half], ALU.add)
                norm_p1(s)

        def moe(l, b, t, tail_cb):
            pb = bank()
            for s in range(NSUB):
                for kb in range(8):
                    mm(pb[:, s * 16:(s + 1) * 16], hT[:, kb, s * 128:(s + 1) * 128], wr[:, kb, :],
                       start=(kb == 0), stop=(kb == 7), last=(kb == 7 and s == NSUB - 1))
            sc_, bi_, sel_, w_ = rt4
            act(sc_, pb[:, 0:64], AF.Sigmoid)
            tt("dve", bi_.re("p (u e) -> p u e", u=4), sc_.re("p (u e) -> p u e", u=4),
               br.re("p (o e) -> p o e", o=1).bc([128, 4, 16]), ALU.add)
            b3 = bi_.re("p (u g e) -> p u g e", u=4, g=4)
            a0, a1, a2, a3 = b3[:, :, :, 0], b3[:, :, :, 1], b3[:, :, :, 2], b3[:, :, :, 3]
            p_, q_, r_, s_, m1, m2, gs, gmx, og, wsum = [v.re("p (u g) -> p u g", u=4) for v in rs4]
            tt("dve", p_, a0, a1, ALU.max)
            tt("dve", q_, a0, a1, ALU.min)
            tt("dve", r_, a2, a3, ALU.max)
            tt("dve", s_, a2, a3, ALU.min)
            tt("dve", m1, p_, r_, ALU.max)
            tt("dve", p_, p_, r_, ALU.min)
            tt("dve", q_, q_, s_, ALU.max)
            tt("dve", m2, p_, q_, ALU.max)
            tt("dve", gs, m1, m2, ALU.add)
            K.op("dve", lambda: nc.vector.tensor_reduce(out=gmx.ap[:, :, 0], in_=gs.ap, axis=AX.X, op=ALU.max),
                 _res(gs), _res(gmx))
            tt("dve", og, gs, gmx[:, :, 0:1].bc([128, 4, 4]), ALU.is_ge)
            sel4 = sel_.re("p (u g e) -> p u g e", u=4, g=4)
            tt("dve", sel4, b3, m2.re("p u (g o) -> p u g o", o=1).bc([128, 4, 4, 4]), ALU.is_ge)
            tt("dve", sel4, sel4, og.re("p u (g o) -> p u g o", o=1).bc([128, 4, 4, 4]), ALU.mult)
            tt("dve", w_, sc_, sel_, ALU.mult)
            K.op("dve", lambda: nc.vector.tensor_reduce(out=wsum.ap[:, :, 0], in_=w_.ap.rearrange("p (u e) -> p u e", u=4),
                                                         axis=AX.X, op=ALU.add),
                 _res(w_), _res(wsum))
            K.op("dve", lambda: nc.vector.reciprocal(out=wsum.ap[:, :, 1], in_=wsum.ap[:, :, 0]),
                 _res(wsum), _res(wsum))
            tt("dve", comb, w_.re("p (u e) -> p u e", u=4), wsum[:, :, 1:2].bc([128, 4, 16]), ALU.mult)
            acc = [BP[s].re("p a c -> p (a c)").bitcast(F32) for s in range(NSUB)]
            def gate_up(e):
                Wg, Wu = wgetn(2)
                heT = BP[4 + e % 2]
                for hb in range(4):
                    pg = bank()
                    for kb in range(8):
                        mm(pg, Wg[:, kb, hb * 128:(hb + 1) * 128], hT[:, kb, :], start=(kb == 0), stop=(kb == 7))
                    pu = bank()
                    for kb in range(8):
                        mm(pu, Wu[:, kb, hb * 128:(hb + 1) * 128], hT[:, kb, :], start=(kb == 0), stop=(kb == 7))
                    sg = tmpb[hb % 3]
                    act(sg, pg, AF.Silu)
                    tt("dve", heT[:, hb, :], sg, pu, ALU.mult)

            def down(e):
                Wd = wget()
                heT = BP[4 + e % 2]
                for s in range(NSUB):
                    for half in range(2):
                        pb = bank()
                        for hb in range(4):
                            mm(pb, heT[:, hb, s * 128:(s + 1) * 128], Wd[:, hb, half * 512:(half + 1) * 512],
                               start=(hb == 0), stop=(hb == 3))
                        hs = slice(half * 512, (half + 1) * 512)
                        if e == 0:
                            ts("dve", acc[s][:, hs], pb, comb[:, s, e:e + 1], ALU.mult)
                        else:
                            stt(acc[s][:, hs], pb, comb[:, s, e:e + 1], acc[s][:, hs], ALU.mult, ALU.add)

            gate_up(0)
            for e in range(16):
                if e < 15:
                    gate_up(e + 1)
                down(e)
            load_gate(l, b, 40)
            for s in range(NSUB):
                tt("dve", acc[s], acc[s], gtb, ALU.mult)
                tt("dve", xt[s], xt[s], acc[s], ALU.add)
                tail_cb(s, 0)
            for s in range(NSUB):
                tail_cb(s, 1)
            for s in range(NSUB):
                tail_cb(s, 2)

        for b in range(2):
            for l in range(2):
                memset("pool", S_a[l][0], 0.0)
                memset("pool", R_b[l], 0.0)
            for t in range(NT):
                for s in range(NSUB):
                    K.dma("sp", xt[s], x_d[b, t * TT + s * 128:t * TT + (s + 1) * 128, :])
                def final_sub(s, phase, b=b, t=t):
                    if phase == 0:
                        act(xb4[s], xt[s], AF.Square, accum=ss[:, 4 + s:5 + s])
                    elif phase == 1:
                        ts("dve", rstd[:, 4 + s:5 + s], ss[:, 4 + s:5 + s], 1.0 / D, ALU.mult, EPS, ALU.add)
                        act(rstd[:, 4 + s:5 + s], rstd[:, 4 + s:5 + s], AF.Ln)
                        act(rstd[:, 4 + s:5 + s], rstd[:, 4 + s:5 + s], AF.Exp, scale=-0.5)
                    else:
                        stt(xt[s], xt[s], rstd[:, 4 + s:5 + s], gfin, ALU.mult, ALU.mult)
                        K.dma("sp", out_sub[s][b, t * TT + s * 128:t * TT + (s + 1) * 128, :], xt[s])
                for l in range(2):
                    mixer(l, b, t, prenormed=(l == 1), tail_cb=None)
                    norm_to_hT(A2T, l, b, 24, p1_done=True, act_subs=(0, 1))
                    if l == 0:
                        moe(l, b, t, tail_cb=norm_p1)
                        norm_to_hT(A1T, 1, b, 0, p1_done=True, act_subs=(0, 1))
                    else:
                        moe(l, b, t, tail_cb=final_sub)
        for s_ in range(NSUB):
            K._wait("sp", {out_sub[s_].res[0].dsem: K.cnt[out_sub[s_].res[0].dsem]})
        if dbg is not None and dbg_d.res[0].dsem is not None:
            K._wait("sp", {dbg_d.res[0].dsem: K.cnt[dbg_d.res[0].dsem]})
        assert wpos[0] == len(sched), (wpos, len(sched))
        print("built: inst=%d waits=%d dsems=%d" % (K.ninst, K.nwait, K.ndsem))
    return nc


def make_consts(S):
    c = {}
    c["c_ident"] = np.eye(128, dtype=np.float32)
    P = np.zeros((128, 128), np.float32)
    for d in range(64):
        P[d + 64, d] = -1.0
        P[d, d + 64] = 1.0
    c["c_prot"] = P
    l = np.arange(128)[:, None]
    j = np.arange(128)[None, :]
    mA = ((l // 32 == j // 32) & (l <= j)).astype(np.float32)
    c["c_maskA"] = np.tile(mA, (1, 4))
    gam = 1.0 - 2.0 ** (-5.0 - np.arange(4, dtype=np.float64))
    sc = 128.0 ** -0.5
    intra = np.zeros((128, 4, 128), np.float64)
    qdec = np.zeros((128, 4, 128), np.float64)
    kdec = np.zeros((128, 4, 128), np.float64)
    same = (l // 64 == j // 64)
    for h in range(4):
        intra[:, h, :] = np.where(same, gam[h] ** np.abs(l - j), 0.0) * sc
        qdec[:, h, :] = (gam[h] ** ((np.arange(128) % 64) + 1.0))[None, :] * sc
        kdec[:, h, :] = (gam[h] ** (63.0 - (np.arange(128) % 64)))[:, None]
    c["c_intra"] = intra.reshape(128, 512).astype(np.float32)
    c["c_qdec"] = qdec.reshape(128, 512).astype(np.float32)
    c["c_kdec"] = kdec.reshape(128, 512).astype(np.float32)
    sm = np.ones((128, 512), np.float32)
    sm[:, ::32] = 0.0
    c["c_smask"] = sm
    half = 64
    inv = 10000.0 ** (-np.arange(half, dtype=np.float32) / half)
    ang = np.arange(S, dtype=np.float32)[None, :] * np.concatenate([inv, inv])[:, None].astype(np.float32)
    c["c_cos"] = np.cos(ang).astype(np.float32)
    c["c_sin"] = np.sin(ang).astype(np.float32)
    return c


def make_in_maps(inp, S, ncore=NCORE):
    f = lambda a: np.ascontiguousarray(np.asarray(a, dtype=np.float32))
    consts = make_consts(S)
    shared = dict(consts)
    shared["w_ada"] = f(inp["w_ada"])
    shared["b_adaT"] = f(np.asarray(inp["b_ada"]).reshape(2, 48, 128).transpose(2, 0, 1))
    shared["g1T"] = f(np.asarray(inp["g_norm1"]).reshape(2, 8, 128).transpose(2, 0, 1))
    shared["g2T"] = f(np.asarray(inp["g_norm2"]).reshape(2, 8, 128).transpose(2, 0, 1))
    shared["w_in"] = f(inp["w_in"])
    shared["lbT"] = f(np.asarray(inp["lb_logits"]).reshape(2, 4, 128).transpose(2, 0, 1))
    shared["ghT"] = f(np.asarray(inp["g_hgrn"]).transpose(2, 0, 1))
    shared["grT"] = f(np.asarray(inp["g_ret"]).reshape(2, 4, 2, 128).transpose(3, 0, 1, 2).reshape(128, 2, 8))
    shared["w_branch_a"] = f(inp["w_branch_a"])
    shared["w_branch_b"] = f(inp["w_branch_b"])
    shared["w_out"] = f(inp["w_out"])
    shared["w_routerT"] = f(np.asarray(inp["w_router"]).reshape(8, 128, 16).transpose(1, 0, 2))
    shared["b_router_bc"] = f(np.broadcast_to(np.asarray(inp["b_router"])[None, :], (128, 16)))
    shared["w_exp_gate"] = f(inp["w_exp_gate"])
    shared["w_exp_up"] = f(inp["w_exp_up"])
    shared["w_exp_down"] = f(inp["w_exp_down"])
    shared["g_final_bc"] = f(np.broadcast_to(np.asarray(inp["g_final"])[None, :], (128, D)))
    x = np.asarray(inp["x"])
    c = np.asarray(inp["c"])
    maps = []
    for i in range(ncore):
        m = dict(shared)
        m["x"] = f(x[2 * i:2 * i + 2, :S])
        m["cT"] = f(c[2 * i:2 * i + 2].T.reshape(8, 128, 2).transpose(1, 0, 2))
        maps.append(m)
    return maps


_NC_CACHE = {}


def run(inputs, ncore=NCORE, dbg=None):
    S = int(np.asarray(inputs["x"]).shape[1])
    key = (S, dbg)
    if key not in _NC_CACHE:
        _NC_CACHE[key] = build(S, dbg)
    nc = _NC_CACHE[key]
    maps = make_in_maps(inputs, S, ncore)
    res = run_bass_kernel_spmd(nc, maps, core_ids=list(range(ncore)))
    out = np.concatenate([np.asarray(r["out"]) for r in res.results], axis=0)
    if dbg is not None:
        return out.astype(np.float32), [np.asarray(r["dbg"]) for r in res.results]
    return out.astype(np.float32)


def kernel(**inputs):
    return run(inputs, NCORE)
```

```python
import numpy as np
from contextlib import ExitStack
import concourse.bass as bass
import concourse.mybir as mybir
from concourse.bass_utils import run_bass_kernel_spmd

F32 = mybir.dt.float32
BF16 = mybir.dt.bfloat16
AF = mybir.ActivationFunctionType
ALU = mybir.AluOpType
AX = mybir.AxisListType

D = 1024
TT = 512
NSUB = 4
EPS = 1e-6
NCORE = 8
NSLOT = 4


class Res:
    __slots__ = ("name", "w", "r", "dsem")

    def __init__(self, name):
        self.name = name
        self.w = None
        self.r = {}
        self.dsem = None


class V:
    __slots__ = ("ap", "res")

    def __init__(self, ap, res):
        self.ap = ap
        self.res = res

    def __getitem__(self, idx):
        return V(self.ap[idx], self.res)

    def v(self, ap):
        return V(ap, self.res)

    def re(self, s, **kw):
        return V(self.ap.rearrange(s, **kw), self.res)

    def bc(self, shape):
        return V(self.ap.to_broadcast(shape), self.res)

    def bitcast(self, dt):
        return V(self.ap.bitcast(dt), self.res)


class Sched:
    def __init__(self, nc, st):
        self.nc = nc
        self.st = st
        self.eng = {"pe": nc.tensor, "act": nc.scalar, "dve": nc.vector, "pool": nc.gpsimd, "sp": nc.sync}
        self.sems = {}
        self.cnt = {}
        self.seen = {k: {} for k in self.eng}
        for k in self.eng:
            self.sems[k] = st.enter_context(nc.semaphore("sem_" + k))
            self.cnt[k] = 0
        self.ndsem = 0
        self.nwait = 0
        self.ninst = 0

    def new_dsem(self):
        key = ("d", self.ndsem)
        self.ndsem += 1
        self.sems[key] = self.st.enter_context(self.nc.semaphore("dsem%d" % key[1]))
        self.cnt[key] = 0
        return key

    def _deps(self, reads, writes, skipkey=None):
        deps = {}

        def add(tok):
            if tok is None:
                return
            k, v = tok
            if k == skipkey:
                return
            if deps.get(k, 0) < v:
                deps[k] = v
        for r in reads:
            add(r.w)
        for w in writes:
            add(w.w)
            for k, v in w.r.items():
                add((k, v))
        return deps

    def _wait(self, E, deps):
        seen = self.seen[E]
        for k, v in deps.items():
            if seen.get(k, 0) >= v:
                continue
            self.eng[E].wait_ge(self.sems[k], v)
            seen[k] = v
            self.nwait += 1

    def _mark(self, tok, reads, writes):
        k, v = tok
        for r in reads:
            if r.r.get(k, 0) < v:
                r.r[k] = v
        for w in writes:
            w.w = tok
            w.r = {}

    def op(self, E, fn, reads, writes, inc=True):
        deps = self._deps(reads, writes)
        if E == "pe":
            deps.pop("pe", None)
        self._wait(E, deps)
        inst = fn()
        self.ninst += 1
        if inc:
            self.cnt[E] += 1
            inst.then_inc(self.sems[E], 1)
            tok = (E, self.cnt[E])
        else:
            tok = (E, self.cnt[E] + 1)
        self._mark(tok, reads, writes)
        return inst

    def dma(self, Q, out, in_, **kw):
        reads, writes = in_.res, out.res
        wres = writes[0]
        if wres.dsem is None:
            wres.dsem = {}
        qk = "sw" if Q == "pool" else "hw"
        if qk not in wres.dsem:
            wres.dsem[qk] = self.new_dsem()
        key = wres.dsem[qk]
        self._wait(Q, self._deps(reads, writes, skipkey=key))
        inst = self.eng[Q].dma_start(out=out.ap, in_=in_.ap, **kw)
        self.ninst += 1
        self.cnt[key] += 16
        inst.then_inc(self.sems[key], 16)
        self._mark((key, self.cnt[key]), reads, writes)


def _res(*vs):
    out = []
    for v in vs:
        if isinstance(v, V):
            for r in v.res:
                if r not in out:
                    out.append(r)
    return out


def _ap(v):
    return v.ap if isinstance(v, V) else v


def build(S=4096, dbg=None):
    NT = S // TT
    nc = bass.Bass("TRN2", target_bir_lowering=False)
    st = ExitStack()
    with st:
        K = Sched(nc, st)

        def din(name, shape, dt=F32):
            return V(nc.dram_tensor(name, list(shape), dt, kind="ExternalInput").ap(), [Res(name)])

        x_d = din("x", [2, S, D])
        cT_d = din("cT", [128, 8, 2])
        w_ada_d = din("w_ada", [2, D, 6 * D])
        b_adaT_d = din("b_adaT", [128, 2, 48])
        g1T_d = din("g1T", [128, 2, 8])
        g2T_d = din("g2T", [128, 2, 8])
        w_in_d = din("w_in", [2, D, 7168])
        lbT_d = din("lbT", [128, 2, 4])
        ghT_d = din("ghT", [128, 2, 4])
        grT_d = din("grT", [128, 2, 8])
        wa_d = din("w_branch_a", [2, 512, D])
        wb_d = din("w_branch_b", [2, D, D])
        wo_d = din("w_out", [2, D, D])
        wr_d = din("w_routerT", [128, 8, 16])
        br_d = din("b_router_bc", [128, 16])
        wg_d = din("w_exp_gate", [2, 16, D, 512])
        wu_d = din("w_exp_up", [2, 16, D, 512])
        wd_d = din("w_exp_down", [2, 16, 512, D])
        gfin_d = din("g_final_bc", [128, D])
        ident_d = din("c_ident", [128, 128])
        prot_d = din("c_prot", [128, 128])
        maskA_d = din("c_maskA", [128, 512])
        intra_d = din("c_intra", [128, 512])
        qdec_d = din("c_qdec", [128, 512])
        kdec_d = din("c_kdec", [128, 512])
        smask_d = din("c_smask", [128, 512])
        cos_d = din("c_cos", [128, S])
        sin_d = din("c_sin", [128, S])
        out_ap = nc.dram_tensor("out", [2, S, D], F32, kind="ExternalOutput").ap()
        out_sub = [V(out_ap, [Res("out%d" % s_)]) for s_ in range(NSUB)]
        scr_d = V(nc.dram_tensor("mod_scr", [2, 48, 2, 128], F32, kind="Internal").ap(), [Res("scr")])
        if dbg is not None:
            dbg_d = V(nc.dram_tensor("dbg", list(dbg), F32, kind="ExternalOutput").ap(), [Res("dbg")])

        def sb(name, shape, dt):
            t = st.enter_context(nc.sbuf_tensor("s_" + name, list(shape), dt))
            return V(t[:], [Res(name)])

        xt = [sb("x%d" % s, [128, D], F32) for s in range(NSUB)]
        hT = sb("hT", [128, 8, TT], BF16)
        ring = [sb("ring%d" % i, [128, 4096], BF16) for i in range(NSLOT)]
        BP = [sb("bp%d" % i, [128, 4, TT], BF16) for i in range(11)]
        Fe = sb("Fe", [128, 4, TT], F32)
        FBt = st.enter_context(nc.sbuf_tensor("s_FB", [128, 6, TT], F32))
        FBres = [Res("FB%d" % i) for i in range(6)]
        FBv = [V(FBt[:, i, :], [FBres[i]]) for i in range(6)]
        F1, F2, F3 = FBv[0:2], FBv[2:4], FBv[4:6]
        GB = [V(FBt[:, 2 * i:2 * i + 2, :].bitcast(BF16).rearrange("p a (k c) -> p (a k) c", k=2), [FBres[2 * i], FBres[2 * i + 1]])
              for i in range(3)]
        kinv_s = [sb("kinv_s%d" % i, [128, 512], BF16) for i in range(2)]
        ATa_s = [sb("ATa_s%d" % i, [128, 512], BF16) for i in range(2)]
        krd_s = [sb("krd_s%d" % i, [128, 512], BF16) for i in range(2)]
        ATb_s = [sb("ATb_s%d" % i, [128, 512], BF16) for i in range(2)]
        xb4 = [sb("xbn%d" % i, [128, D], BF16) for i in range(4)]
        xb = xb4
        tmpf = [sb("tmpf%d" % i, [128, TT], F32) for i in range(4)]
        tmpb = [sb("tmpb%d" % i, [128, TT], BF16) for i in range(3)]
        S_a = [[sb("S_a%d_%d" % (l, i), [128, 512], F32) for i in range(2)] for l in range(2)]
        R_b = [sb("R_b%d" % l, [128, 1024], F32) for l in range(2)]
        S_bf = [sb("S_bf%d" % i, [128, 512], BF16) for i in range(4)]
        R_bf = [sb("R_bf%d" % i, [128, 1024], BF16) for i in range(3)]
        stmp = sb("stmp", [128, 512], F32)
        gtb = sb("gtb", [128, D], F32)
        gfin = sb("gfin", [128, D], F32)
        cosS = sb("cosS", [128, TT], F32)
        sinS = sb("sinS", [128, TT], F32)
        ident = sb("ident", [128, 128], BF16)
        prot = sb("prot", [128, 128], BF16)
        ones = sb("ones", [128, 128], BF16)
        maskA = sb("maskA", [128, 512], F32)
        intra = sb("intra", [128, 512], F32)
        qdec = sb("qdec", [128, 512], F32)
        kdec = sb("kdec", [128, 512], F32)
        smask = sb("smask", [128, 512], F32)
        cT = sb("cT", [128, 8, 2], F32)
        ca = sb("ca", [128, 8, 2], F32)
        b_adaT = sb("b_adaT", [128, 2, 48], F32)
        modT = sb("modT", [128, 2, 48, 2], F32)
        modrow = sb("modrow", [96, 128], F32)
        g1T = sb("g1T", [128, 2, 8], F32)
        g2T = sb("g2T", [128, 2, 8], F32)
        A1T = sb("A1T", [128, 2, 2, 8], F32)
        A2T = sb("A2T", [128, 2, 2, 8], F32)
        lbT = sb("lbT", [128, 2, 4], F32)
        oml = sb("oml", [128, 2, 4], F32)
        ghT = sb("ghT", [128, 2, 4], F32)
        grT = sb("grT", [128, 2, 8], F32)
        wr = sb("wr", [128, 8, 16], BF16)
        br = sb("br", [128, 16], F32)
        ss = sb("ss", [128, 8], F32)
        rstd = sb("rstd", [128, 8], F32)
        comb = sb("comb", [128, NSUB, 16], F32)
        rt4 = [sb("rt%d" % i, [128, 64], F32) for i in range(4)]
        rs4 = [sb("rs%d" % i, [128, 16], F32) for i in range(10)]
        rs = rs4

        if dbg is not None:
            tapbuf = sb("tapbuf", [128, 512], F32)
        pst = st.enter_context(nc.psum_tensor("ps", [128, 8, 512], F32))
        psres = [Res("ps%d" % i) for i in range(8)]
        pscur = [0]

        def bank(n=1):
            i = pscur[0]
            if n == 2 and i % 2 == 1:
                i = (i + 1) % 8
            pscur[0] = (i + n) % 8
            if n == 1:
                return V(pst[:, i, :], [psres[i]])
            return V(pst[:, i:i + 2, :], [psres[i], psres[i + 1]])

        def mm(out, lhsT, rhs, start=True, stop=True, last=None, tp=None):
            if last is None:
                last = stop
            kw = {}
            if tp is not None:
                kw["tile_position"] = tp
            K.op("pe", lambda: nc.tensor.matmul(out.ap, lhsT.ap, rhs.ap, start=start, stop=stop, **kw),
                 _res(lhsT, rhs), _res(out), inc=last)

        def tr(out, in_, idv, last=True):
            K.op("pe", lambda: nc.tensor.transpose(out.ap, in_.ap, idv.ap), _res(in_, idv), _res(out), inc=last)

        def act(out, in_, func, scale=None, bias=None, accum=None):
            kw = {}
            if scale is not None:
                kw["scale"] = _ap(scale)
            if bias is not None:
                kw["bias"] = _ap(bias)
            if accum is not None:
                kw["accum_out"] = accum.ap
            K.op("act", lambda: nc.scalar.activation(out=out.ap, in_=in_.ap, func=func, **kw),
                 _res(in_, scale, bias), _res(out, accum))

        def tt(E, out, in0, in1, op):
            K.op(E, lambda: K.eng[E].tensor_tensor(out=out.ap, in0=in0.ap, in1=in1.ap, op=op),
                 _res(in0, in1), _res(out))

        def ts(E, out, in0, s1, op0, s2=None, op1=None):
            if E == "pool" and op1 is None:
                s2, op1 = 0.0, ALU.add

            def f():
                if op1 is None:
                    return K.eng[E].tensor_scalar(out=out.ap, in0=in0.ap, scalar1=_ap(s1), scalar2=None, op0=op0)
                return K.eng[E].tensor_scalar(out=out.ap, in0=in0.ap, scalar1=_ap(s1), scalar2=_ap(s2),
                                              op0=op0, op1=op1)
            K.op(E, f, _res(in0, s1, s2), _res(out))

        def stt(out, in0, scalar, in1, op0, op1):
            K.op("dve", lambda: nc.vector.scalar_tensor_tensor(out=out.ap, in0=in0.ap, scalar=_ap(scalar),
                                                                in1=in1.ap, op0=op0, op1=op1),
                 _res(in0, scalar, in1), _res(out))

        def cp(E, out, in_):
            if E == "act":
                K.op("act", lambda: nc.scalar.copy(out=out.ap, in_=in_.ap), _res(in_), _res(out))
            else:
                K.op(E, lambda: K.eng[E].tensor_copy(out=out.ap, in_=in_.ap), _res(in_), _res(out))

        def memset(E, out, val):
            K.op(E, lambda: K.eng[E].memset(out.ap, val), [], _res(out))

        ident_f = Fe[:, 0, 0:128]
        prot_f = Fe[:, 1, 0:128]
        wr_f = Fe[:, 2, 0:128].re("p (k e) -> p k e", k=8)
        epsb_t = sb("epsb", [128, 1], F32)
        memset("dve", epsb_t, EPS)
        epsb = epsb_t
        for dst, src in ((ident_f, ident_d), (prot_f, prot_d), (maskA, maskA_d), (intra, intra_d), (qdec, qdec_d),
                         (kdec, kdec_d), (smask, smask_d), (cT, cT_d), (b_adaT, b_adaT_d), (g1T, g1T_d),
                         (g2T, g2T_d), (lbT, lbT_d), (ghT, ghT_d), (grT, grT_d), (wr_f, wr_d), (br, br_d),
                         (gfin, gfin_d)):
            K.dma("sp", dst, src)
        cp("dve", ident, ident_f)
        cp("dve", prot, prot_f)
        cp("dve", wr, wr_f)
        memset("dve", ones, 1.0)
        memset("dve", oml[:, 0, :], 1.0)
        tt("dve", rs[0][:, 0:4], lbT[:, 0, :], lbT[:, 1, :], ALU.subtract)
        act(oml[:, 1, :], rs[0][:, 0:4], AF.Sigmoid)
        act(ca, cT, AF.Silu)

        sched = []

        def wslice(src_rows_ap, kb, cols):
            sched.append((src_rows_ap, kb, cols))

        ZC = dict(qa=0, fa=512, ia=1024, oga=1536, qb=2048, kb=2560, vb=3072, ogb=4096, ma=5120, mb=6144)
        for b in range(2):
            for t in range(NT):
                for l in range(2):
                    def wi(c0, l=l):
                        wslice(w_in_d[l][:, c0:c0 + 512].re("(k p) c -> p k c", p=128), 8, 512)
                    for c0 in (ZC["qa"], ZC["fa"], ZC["ia"]):
                        wi(c0)
                    for c0 in (ZC["qb"], ZC["kb"], ZC["vb"], ZC["vb"] + 512):
                        wi(c0)
                    for c0 in (ZC["ma"], ZC["ma"] + 512, ZC["mb"]):
                        wi(c0)
                    for c0 in (ZC["oga"], ZC["ogb"], ZC["ogb"] + 512):
                        wi(c0)
                    wi(ZC["mb"] + 512)
                    wslice(wa_d[l].re("(k p) c -> p k c", p=128), 4, 1024)
                    wslice(wb_d[l][:, 0:512].re("(k p) c -> p k c", p=128), 8, 512)
                    wslice(wb_d[l][:, 512:1024].re("(k p) c -> p k c", p=128), 8, 512)
                    wslice(wo_d[l][:, 0:512].re("(k p) c -> p k c", p=128), 8, 512)
                    wslice(wo_d[l][:, 512:1024].re("(k p) c -> p k c", p=128), 8, 512)
                    wslice(wg_d[l, 0].re("(k p) c -> p k c", p=128), 8, 512)
                    wslice(wu_d[l, 0].re("(k p) c -> p k c", p=128), 8, 512)
                    for e in range(16):
                        if e < 15:
                            wslice(wg_d[l, e + 1].re("(k p) c -> p k c", p=128), 8, 512)
                            wslice(wu_d[l, e + 1].re("(k p) c -> p k c", p=128), 8, 512)
                        wslice(wd_d[l, e].re("(k p) c -> p k c", p=128), 4, 1024)
        wpos = [0, 0]

        NSL = len(sched) // (2 * NT * 2)
        assert NSL * 2 * NT * 2 == len(sched)
        wscr_ap = nc.dram_tensor("w_scr", [2 * NSL, 128, 4096], BF16, kind="Internal").ap()
        scrw_res = [Res("scrw%d" % i) for i in range(NSLOT)]

        def wgetn(n):
            i = wpos[0]
            assert n <= NSLOT
            wpos[0] += n
            while wpos[1] < len(sched) and wpos[1] <= i + NSLOT - 1:
                j = wpos[1]
                src, kb, cols = sched[j]
                tl, k = divmod(j, NSL)
                lyr = tl % 2
                slot = ring[j % NSLOT]
                if tl < 2:
                    K.dma("pool", slot.re("p (k c) -> p k c", k=kb), src)
                    K.dma("sp", V(wscr_ap[lyr * NSL + k], [scrw_res[j % NSLOT]]), slot)
                else:
                    j0 = lyr * NSL + k
                    K.dma("sp", slot, V(wscr_ap[lyr * NSL + k], [scrw_res[j0 % NSLOT]]))
                wpos[1] += 1
            return [ring[j % NSLOT].re("p (k c) -> p k c", k=sched[j][1]) for j in range(i, i + n)]

        def wget():
            return wgetn(1)[0]

        adaring = [ring[i].bitcast(F32).re("p (k c) -> p k c", k=8) for i in range(2)]
        for l in range(2):
            pb = bank()
            for s in range(24):
                wsl = adaring[s % 2]
                K.dma("sp", wsl, w_ada_d[l][:, s * 256:(s + 1) * 256].re("(k p) c -> p k c", p=128))
                for cbk in range(2):
                    blk = s * 2 + cbk
                    for kb in range(8):
                        mm(pb[:, blk * 2:blk * 2 + 2], wsl[:, kb, cbk * 128:(cbk + 1) * 128], ca[:, kb, :],
                           start=(kb == 0), stop=(kb == 7))
            tt("dve", modT[:, l], pb[:, 0:96].re("p (k b) -> p k b", b=2),
               b_adaT[:, l, :].re("p (k o) -> p k o", o=1).bc([128, 48, 2]), ALU.add)
            for b in range(2):
                stt(A1T[:, l, b, :], modT[:, l, 8:16, b], 1.0, g1T[:, l, :], ALU.add, ALU.mult)
                stt(A2T[:, l, b, :], modT[:, l, 32:40, b], 1.0, g2T[:, l, :], ALU.add, ALU.mult)
            pb2 = bank()
            tr(pb2[:96, :128], modT[:, l].re("p k b -> p (k b)"), ident_f)
            cp("dve", modrow, pb2[:96, :128])
            K.dma("sp", scr_d[l].re("k b p -> (k b) p"), modrow)

        def load_gate(l, b, blk0):
            src = scr_d[l, blk0:blk0 + 8, b, :]
            K.dma("sp", gtb.re("p (k c) -> p k c", k=8), V(src.ap.partition_broadcast(128), src.res))

        def norm_p1(s, phase=None):
            if phase in (None, 0):
                act(xb4[s], xt[s], AF.Square, accum=ss[:, s:s + 1])
            if phase in (None, 1):
                ts("dve", rstd[:, s:s + 1], ss[:, s:s + 1], 1.0 / D, ALU.mult, EPS, ALU.add)
                act(rstd[:, s:s + 1], rstd[:, s:s + 1], AF.Ln)
                act(rstd[:, s:s + 1], rstd[:, s:s + 1], AF.Exp, scale=-0.5)
            if phase in (None, 2):
                ts("pool", xb4[s], xt[s], rstd[:, s:s + 1], ALU.mult)

        def norm_p2(AT, l, b, shblk, s, on_act):
            pb = bank().bitcast(BF16)
            for kb in range(8):
                tr(pb[:, kb * 128:(kb + 1) * 128], xb4[s][:, kb * 128:(kb + 1) * 128], ident, last=(kb == 7))
            if on_act:
                for kb in range(8):
                    act(hT[:, kb, s * 128:(s + 1) * 128], pb[:, kb * 128:(kb + 1) * 128], AF.Identity,
                        scale=AT[:, l, b, kb:kb + 1], bias=modT[:, l, shblk + kb, b:b + 1])
            else:
                for hf in range(2):
                    hv = tmpf[hf].re("p (k j) -> p k j", k=4)
                    tt("dve", hv, pb[:, hf * 512:(hf + 1) * 512].re("p (k j) -> p k j", k=4),
                       AT[:, l, b, 4 * hf:4 * hf + 4].re("p (k o) -> p k o", o=1).bc([128, 4, 128]), ALU.mult)
                    tt("dve", hT[:, 4 * hf:4 * hf + 4, s * 128:(s + 1) * 128], hv,
                       modT[:, l, shblk + 4 * hf:shblk + 4 * hf + 4, b:b + 1].bc([128, 4, 128]), ALU.add)

        def norm_to_hT(AT, l, b, shblk, p1_done=False, act_subs=(0,)):
            if not p1_done:
                for s in range(NSUB):
                    norm_p1(s)
            for s in range(NSUB):
                norm_p2(AT, l, b, shblk, s, s in act_subs)

        def proj_fm(W, nblk, evac):
            for blk in range(nblk):
                pb = bank()
                for kb in range(8):
                    mm(pb, W[:, kb, blk * 128:(blk + 1) * 128], hT[:, kb, :], start=(kb == 0), stop=(kb == 7))
                evac(blk, pb)

        def proj_tm(W, evac):
            for s in range(NSUB):
                pb = bank()
                for kb in range(8):
                    mm(pb, hT[:, kb, s * 128:(s + 1) * 128], W[:, kb, :], start=(kb == 0), stop=(kb == 7))
                evac(s, pb)

        tapidx = [0]
        tapnames = []

        def tap(name, v, cond=True):
            if dbg is None or not cond:
                return
            i = tapidx[0]
            if i >= dbg[0]:
                return
            tapidx[0] += 1
            tapnames.append(name)
            n = v.ap.shape[-1] if len(v.ap.shape) == 2 else None
            if v.ap.dtype != F32:
                cp("dve", tapbuf[:, 0:n], v)
                v = tapbuf[:, 0:n]
            K.dma("sp", dbg_d[i, 0:v.ap.shape[0], 0:n], v)
        build.tapnames = tapnames

        def mixer(l, b, t, prenormed, tail_cb):
            tok0 = t * TT
            T0 = (l == 0 and b == 0 and t == 0)
            K.dma("sp", cosS, cos_d[:, tok0:tok0 + TT])
            K.dma("sp", sinS, sin_d[:, tok0:tok0 + TT])
            if not prenormed:
                norm_to_hT(A1T, l, b, 0)
            qeT, kinvT, va_tm, oaT, qrT, krT, qdT = BP[0:7]
            vb_tm = [BP[7], BP[8]]
            obT = [BP[9], BP[10]]
            qsT = BP[9]
            W = wget()
            proj_fm(W, 4, lambda blk, pb: act(qsT[:, blk, :], pb, AF.Silu))
            W = wget()

            pending = []

            def pump(n):
                for _ in range(n):
                    if not pending:
                        return
                    g = pending[0]
                    try:
                        next(g)
                    except StopIteration:
                        pending.pop(0)

            def chain_f(blk, f1, f2, f3):
                act(f2, f1, AF.Ln, bias=1.0)
                yield
                act(f2, f2, AF.Exp, scale=-1.0)
                ts("dve", f1, f2, oml[:, l, blk:blk + 1], ALU.mult)
                yield
                act(f2, f1, AF.Ln, scale=-1.0, bias=1.0)
                K.op("dve", lambda: nc.vector.tensor_tensor_scan(out=f3.ap, data0=smask.ap, data1=f2.ap,
                                                                  initial=0.0, op0=ALU.mult, op1=ALU.add),
                     _res(smask, f2), _res(f3))
                yield
                act(Fe[:, blk, :], f3, AF.Exp)
                yield
                act(f2, f3, AF.Exp, scale=-1.0)
                tt("dve", kinvT[:, blk, :], f1, f2, ALU.mult)
                tt("dve", qeT[:, blk, :], qsT[:, blk, :], Fe[:, blk, :], ALU.mult)

            def ev_f(blk, pb):
                f1, f2, f3 = F1[blk % 2], F2[blk % 2], F3[blk % 2]
                while len(pending) >= 2:
                    pump(1)
                act(f1, pb, AF.Exp)
                pending.append(chain_f(blk, f1, f2, f3))
                pump(1)
            proj_fm(W, 4, ev_f)
            W = wget()

            def ev_ia(s, pb):
                cp("act", va_tm[:, s, :], pb)
                pump(2)
            proj_tm(W, ev_ia)
            rot_defer = []
            for which in range(2):
                W = wget()

                def rot_rest(blk, xbf, which):
                    pr_ = bank()
                    mm(pr_, prot, xbf)
                    ta, tb = tmpf[2 * (blk % 2)], tmpf[2 * (blk % 2) + 1]
                    tt("dve", ta, xbf, cosS, ALU.mult)
                    tt("dve", tb, pr_, sinS, ALU.mult)
                    dst = qrT if which == 0 else krT
                    tt("pool", dst[:, blk, :], ta, tb, ALU.add)
                    if which == 0:
                        tt("dve", qdT[:, blk, :].re("p (r j) -> p r j", r=4), dst[:, blk, :].re("p (r j) -> p r j", r=4),
                           qdec[:, blk * 128:(blk + 1) * 128].re("p (o j) -> p o j", o=1).bc([128, 4, 128]), ALU.mult)

                def ev_rot(blk, pb, which=which):
                    xbf = tmpb[1 + (blk % 2)]
                    if rot_defer:
                        rot_defer.pop(0)()
                    cp("act", xbf, pb)
                    rot_defer.append(lambda blk=blk, xbf=xbf, which=which: rot_rest(blk, xbf, which))
                    pump(2)
                proj_fm(W, 4, ev_rot)
            for half in range(2):
                W = wget()

                def ev_vb(s, pb, half=half):
                    while rot_defer:
                        rot_defer.pop(0)()
                    cp("act", vb_tm[half][:, s, :], pb)
                    pump(2)
                proj_tm(W, ev_vb)
            pump(1000)
            assert not pending
            tap("qeT_0", qeT[:, 0, :], T0)

            def vbv(s, h, vb, rows=slice(0, 128)):
                c0 = h * 256 + vb * 128
                return vb_tm[c0 // 512][rows, s, (c0 % 512):(c0 % 512) + 128]

            gam64 = [float((1.0 - 2.0 ** (-5.0 - h)) ** 64) for h in range(4)]
            def fill_gen():
                for dst in GB:
                    Wf = wget()
                    for blk in range(4):
                        pf = bank()
                        for kb in range(8):
                            mm(pf, Wf[:, kb, blk * 128:(blk + 1) * 128], hT[:, kb, :], start=(kb == 0), stop=(kb == 7))
                        cp("act", dst[:, blk, :], pf)
                        yield
            fg = fill_gen()

            def fill():
                next(fg, None)
            for s in range(NSUB):
                tk = slice(s * 128, (s + 1) * 128)
                kinv_tm, ATa, krd_tm, ATb = kinv_s[s % 2], ATa_s[s % 2], krd_s[s % 2], ATb_s[s % 2]
                pb = bank().bitcast(BF16)
                for h in range(4):
                    tr(pb[:, h * 128:(h + 1) * 128], kinvT[:, h, tk], ident, last=(h == 3))
                cp("act", kinv_tm, pb[:, 0:512])
                pb = bank()
                for h in range(4):
                    mm(pb[:, h * 128:(h + 1) * 128], kinvT[:, h, tk], qeT[:, h, tk], last=(h == 3))
                tt("dve", ATa, pb, maskA, ALU.mult)
                pb = bank()
                for h in range(4):
                    mm(pb[:, h * 128:(h + 1) * 128], krT[:, h, tk], qrT[:, h, tk], last=(h == 3))
                tt("dve", ATb, pb, intra, ALU.mult)
                pb = bank().bitcast(BF16)
                for h in range(4):
                    tr(pb[:, h * 128:(h + 1) * 128], krT[:, h, tk], ident, last=(h == 3))
                tt("dve", krd_tm, pb[:, 0:512], kdec, ALU.mult)
                fill()
                sbfs = []
                for cc in range(4):
                    c = s * 4 + cc
                    sbf = S_bf[c % 4]
                    Scur, Snxt = S_a[l][c % 2], S_a[l][(c + 1) % 2]
                    cp("act", sbf, Scur)
                    sbfs.append(sbf)
                    pr = slice(32 * cc, 32 * cc + 32)
                    pb = bank()
                    for h in range(4):
                        hs = slice(h * 128, (h + 1) * 128)
                        mm(pb[:, hs], kinv_tm[pr, hs], va_tm[pr, s, hs], last=(h == 3), tp=(32 * cc, 0))
                    tt("dve", stmp, Scur, pb, ALU.add)
                    col = s * 128 + 32 * cc + 31
                    tt("dve", Snxt.re("p (h v) -> p h v", h=4), stmp.re("p (h v) -> p h v", h=4),
                       Fe[:, :, col:col + 1].bc([128, 4, 128]), ALU.mult)
                rbfs = []
                for cc in range(2):
                    c = s * 2 + cc
                    rbf = R_bf[c % 3]
                    cp("act", rbf, R_b[l])
                    rbfs.append(rbf)
                    pr = slice(64 * cc, 64 * cc + 64)
                    pu = bank(2)
                    for h in range(4):
                        c0 = h * 256
                        mm(pu[:, h // 2, (h % 2) * 256:(h % 2) * 256 + 256], krd_tm[pr, h * 128:(h + 1) * 128],
                           vb_tm[c0 // 512][pr, s, (c0 % 512):(c0 % 512) + 256], last=(h == 3), tp=(64 * cc, 0))
                    for h in range(4):
                        stt(R_b[l][:, h * 256:(h + 1) * 256], R_b[l][:, h * 256:(h + 1) * 256], gam64[h],
                            pu[:, h // 2, (h % 2) * 256:(h % 2) * 256 + 256], ALU.mult, ALU.add)
                fill()
                po = bank()
                for h in range(4):
                    hs = slice(h * 128, (h + 1) * 128)
                    mm(po[:, hs], va_tm[:, s, hs], ATa[:, hs], start=True, stop=False)
                    for cc in range(4):
                        o0 = h * 128 + 32 * cc
                        mm(po[:, o0:o0 + 32], sbfs[cc][:, hs], qeT[:, h, s * 128 + 32 * cc:s * 128 + 32 * cc + 32],
                           start=False, stop=(cc == 3), last=(cc == 3 and h == 3))
                act(tmpb[0], po, AF.Square)
                po2 = bank(2)
                for h in range(4):
                    for vb in range(2):
                        o0 = ((h % 2) * 2 + vb) * 128
                        mm(po2[:, h // 2, o0:o0 + 128], vbv(s, h, vb), ATb[:, h * 128:(h + 1) * 128],
                           start=True, stop=False)
                        for cc in range(2):
                            c0 = h * 256 + vb * 128
                            mm(po2[:, h // 2, o0 + 64 * cc:o0 + 64 * cc + 64], rbfs[cc][:, c0:c0 + 128],
                               qdT[:, h, s * 128 + 64 * cc:s * 128 + 64 * cc + 64], start=False, stop=(cc == 1),
                               last=(cc == 1 and vb == 1 and h == 3))
                ob16 = [xb[0][:, 0:512], xb[0][:, 512:1024]]
                sq = [xb[1][:, 0:512], xb[1][:, 512:1024]]
                for k in range(2):
                    cp("act", ob16[k], po2[:, k, :])
                    act(sq[k], po2[:, k, :], AF.Square)
                fill()
                pn = bank()
                mm(pn, ones, tmpb[0])
                p1 = bank()
                p2 = bank()
                for h in range(4):
                    k, hh = h // 2, h % 2
                    for vb in range(2):
                        o0 = (hh * 2 + vb) * 128
                        mm(p1[:, h * 128:(h + 1) * 128], ones, ob16[k][:, o0:o0 + 128], start=(vb == 0),
                           stop=(vb == 1), last=False)
                for h in range(4):
                    k, hh = h // 2, h % 2
                    for vb in range(2):
                        o0 = (hh * 2 + vb) * 128
                        mm(p2[:, h * 128:(h + 1) * 128], ones, sq[k][:, o0:o0 + 128], start=(vb == 0),
                           stop=(vb == 1), last=(vb == 1 and h == 3))
                rs_a, mean, var = tmpf[0], tmpf[1], tmpf[2]
                act(rs_a, pn, AF.Ln, scale=1.0 / 128, bias=epsb)
                act(mean, p1, AF.Copy, scale=1.0 / 256)
                act(var, p1, AF.Square, scale=1.0 / 256)
                stt(var, p2, 1.0 / 256, var, ALU.mult, ALU.subtract)
                act(var, var, AF.Ln, bias=epsb)
                act(rs_a, rs_a, AF.Exp, scale=-0.5)
                act(var, var, AF.Exp, scale=-0.5)
                tt("dve", oaT[:, :, tk], po.re("p (h j) -> p h j", h=4), rs_a.re("p (h j) -> p h j", h=4), ALU.mult)
                for k in range(2):
                    ta = tmpf[3]
                    mv = mean[:, k * 256:(k + 1) * 256].re("p (h o j) -> p h o j", h=2, o=1).bc([128, 2, 2, 128])
                    rv = var[:, k * 256:(k + 1) * 256].re("p (h o j) -> p h o j", h=2, o=1).bc([128, 2, 2, 128])
                    tt("dve", ta.re("p (h v j) -> p h v j", h=2, v=2), po2[:, k, :].re("p (h v j) -> p h v j", h=2, v=2),
                       mv, ALU.subtract)
                    tt("pool", obT[k][:, :, tk].re("p (h v) j -> p h v j", h=2), ta.re("p (h v j) -> p h v j", h=2, v=2),
                       rv, ALU.mult)
            for _ in range(12):
                fill()
            W = wget()

            def ev_oga(blk, pb):
                sg = tmpb[1 + blk % 2]
                act(sg, pb, AF.Silu)
                stt(oaT[:, blk, :], sg, ghT[:, l, blk:blk + 1], oaT[:, blk, :], ALU.mult, ALU.mult)
            proj_fm(W, 4, ev_oga)
            for half in range(2):
                W = wget()

                def ev_ogb(blk, pb, half=half):
                    sg = tmpb[1 + blk % 2]
                    act(sg, pb, AF.Silu)
                    stt(obT[half][:, blk, :], sg, grT[:, l, half * 4 + blk:half * 4 + blk + 1], obT[half][:, blk, :],
                        ALU.mult, ALU.mult)
                proj_fm(W, 4, ev_ogb)
            tap("oaT_0", oaT[:, 0, :], T0)
            tap("oaT_3", oaT[:, 3, :], T0)
            tap("qrT_0", qrT[:, 0, :], T0)
            tap("krT_1", krT[:, 1, :], T0)
            tap("obT_0", obT[0][:, 0, :], T0)
            tap("obT_7", obT[1][:, 3, :], T0)
            yT = [BP[5], BP[6]]
            W = wget()
            proj_fm(W, 4, lambda blk, pb: cp("act", BP[4][:, blk, :], pb))
            graw_a = [GB[0], GB[1]]
            graw_b = [GB[2], BP[4]]
            Wa, Wb0_, Wb1_ = wgetn(3)
            Wb = [Wb0_, Wb1_]
            for cb in range(8):
                pa = bank()
                for vb in range(4):
                    mm(pa, Wa[:, vb, cb * 128:(cb + 1) * 128], oaT[:, vb, :], start=(vb == 0), stop=(vb == 3))
                pb = bank()
                for kb in range(8):
                    mm(pb, Wb[cb // 4][:, kb, (cb % 4) * 128:(cb % 4) * 128 + 128], obT[kb // 4][:, kb % 4, :],
                       start=(kb == 0), stop=(kb == 7))
                act(tmpb[1], graw_a[cb // 4][:, cb % 4, :], AF.Sigmoid)
                act(tmpb[2], graw_b[cb // 4][:, cb % 4, :], AF.Sigmoid)
                tt("dve", tmpf[0], pa, tmpb[1], ALU.mult)
                tt("dve", tmpf[1], pb, tmpb[2], ALU.mult)
                tt("pool", yT[cb // 4][:, cb % 4, :], tmpf[0], tmpf[1], ALU.add)
            tap("yT_0", yT[0][:, 0, :], T0)
            tap("yT_7", yT[1][:, 3, :], T0)
            load_gate(l, b, 16)
            tap("gt1", gtb, T0)
            Wo = wgetn(2)
            for s in range(NSUB):
                for half in range(2):
                    pb = bank()
                    for kb in range(8):
                        mm(pb, yT[kb // 4][:, kb % 4, s * 128:(s + 1) * 128], Wo[half][:, kb, :],
                           start=(kb == 0), stop=(kb == 7))
                    hs = slice(half * 512, (half + 1) * 512)
                    tt("dve", tmpf[2 + half], pb, gtb[:, hs], ALU.mult)
                    tt("pool" if half == 0 else "dve", xt[s][:, hs], xt[s][:, hs], tmpf[2 + half], ALU.add)
                norm_p1(s)

        def moe(l, b, t, tail_cb):
            pb = bank()
            for s in range(NSUB):
                for kb in range(8):
                    mm(pb[:, s * 16:(s + 1) * 16], hT[:, kb, s * 128:(s + 1) * 128], wr[:, kb, :],
                       start=(kb == 0), stop=(kb == 7), last=(kb == 7 and s == NSUB - 1))
            sc_, bi_, sel_, w_ = rt4
            act(sc_, pb[:, 0:64], AF.Sigmoid)
            tt("dve", bi_.re("p (u e) -> p u e", u=4), sc_.re("p (u e) -> p u e", u=4),
               br.re("p (o e) -> p o e", o=1).bc([128, 4, 16]), ALU.add)
            b3 = bi_.re("p (u g e) -> p u g e", u=4, g=4)
            a0, a1, a2, a3 = b3[:, :, :, 0], b3[:, :, :, 1], b3[:, :, :, 2], b3[:, :, :, 3]
            p_, q_, r_, s_, m1, m2, gs, gmx, og, wsum = [v.re("p (u g) -> p u g", u=4) for v in rs4]
            tt("dve", p_, a0, a1, ALU.max)
            tt("dve", q_, a0, a1, ALU.min)
            tt("dve", r_, a2, a3, ALU.max)
            tt("dve", s_, a2, a3, ALU.min)
            tt("dve", m1, p_, r_, ALU.max)
            tt("dve", p_, p_, r_, ALU.min)
            tt("dve", q_, q_, s_, ALU.max)
            tt("dve", m2, p_, q_, ALU.max)
            tt("dve", gs, m1, m2, ALU.add)
            K.op("dve", lambda: nc.vector.tensor_reduce(out=gmx.ap[:, :, 0], in_=gs.ap, axis=AX.X, op=ALU.max),
                 _res(gs), _res(gmx))
            tt("dve", og, gs, gmx[:, :, 0:1].bc([128, 4, 4]), ALU.is_ge)
            sel4 = sel_.re("p (u g e) -> p u g e", u=4, g=4)
            tt("dve", sel4, b3, m2.re("p u (g o) -> p u g o", o=1).bc([128, 4, 4, 4]), ALU.is_ge)
            tt("dve", sel4, sel4, og.re("p u (g o) -> p u g o", o=1).bc([128, 4, 4, 4]), ALU.mult)
            tt("dve", w_, sc_, sel_, ALU.mult)
            K.op("dve", lambda: nc.vector.tensor_reduce(out=wsum.ap[:, :, 0], in_=w_.ap.rearrange("p (u e) -> p u e", u=4),
                                                         axis=AX.X, op=ALU.add),
                 _res(w_), _res(wsum))
            K.op("dve", lambda: nc.vector.reciprocal(out=wsum.ap[:, :, 1], in_=wsum.ap[:, :, 0]),
                 _res(wsum), _res(wsum))
            tt("dve", comb, w_.re("p (u e) -> p u e", u=4), wsum[:, :, 1:2].bc([128, 4, 16]), ALU.mult)
            acc = [BP[s].re("p a c -> p (a c)").bitcast(F32) for s in range(NSUB)]
            def gate_up(e):
                Wg, Wu = wgetn(2)
                heT = BP[4 + e % 2]
                for hb in range(4):
                    pg = bank()
                    for kb in range(8):
                        mm(pg, Wg[:, kb, hb * 128:(hb + 1) * 128], hT[:, kb, :], start=(kb == 0), stop=(kb == 7))
                    pu = bank()
                    for kb in range(8):
                        mm(pu, Wu[:, kb, hb * 128:(hb + 1) * 128], hT[:, kb, :], start=(kb == 0), stop=(kb == 7))
                    sg = tmpb[hb % 3]
                    act(sg, pg, AF.Silu)
                    tt("dve", heT[:, hb, :], sg, pu, ALU.mult)

            def down(e):
                Wd = wget()
                heT = BP[4 + e % 2]
                for s in range(NSUB):
                    for half in range(2):
                        pb = bank()
                        for hb in range(4):
                            mm(pb, heT[:, hb, s * 128:(s + 1) * 128], Wd[:, hb, half * 512:(half + 1) * 512],
                               start=(hb == 0), stop=(hb == 3))
                        hs = slice(half * 512, (half + 1) * 512)
                        if e == 0:
                            ts("dve", acc[s][:, hs], pb, comb[:, s, e:e + 1], ALU.mult)
                        else:
                            stt(acc[s][:, hs], pb, comb[:, s, e:e + 1], acc[s][:, hs], ALU.mult, ALU.add)

            gate_up(0)
            for e in range(16):
                if e < 15:
                    gate_up(e + 1)
                down(e)
            load_gate(l, b, 40)
            for s in range(NSUB):
                tt("dve", acc[s], acc[s], gtb, ALU.mult)
                tt("dve", xt[s], xt[s], acc[s], ALU.add)
                tail_cb(s, 0)
            for s in range(NSUB):
                tail_cb(s, 1)
            for s in range(NSUB):
                tail_cb(s, 2)

        for b in range(2):
            for l in range(2):
                memset("pool", S_a[l][0], 0.0)
                memset("pool", R_b[l], 0.0)
            for t in range(NT):
                for s in range(NSUB):
                    K.dma("sp", xt[s], x_d[b, t * TT + s * 128:t * TT + (s + 1) * 128, :])
                def final_sub(s, phase, b=b, t=t):
                    if phase == 0:
                        act(xb4[s], xt[s], AF.Square, accum=ss[:, 4 + s:5 + s])
                    elif phase == 1:
                        ts("dve", rstd[:, 4 + s:5 + s], ss[:, 4 + s:5 + s], 1.0 / D, ALU.mult, EPS, ALU.add)
                        act(rstd[:, 4 + s:5 + s], rstd[:, 4 + s:5 + s], AF.Ln)
                        act(rstd[:, 4 + s:5 + s], rstd[:, 4 + s:5 + s], AF.Exp, scale=-0.5)
                    else:
                        stt(xt[s], xt[s], rstd[:, 4 + s:5 + s], gfin, ALU.mult, ALU.mult)
                        K.dma("sp", out_sub[s][b, t * TT + s * 128:t * TT + (s + 1) * 128, :], xt[s])
                for l in range(2):
                    mixer(l, b, t, prenormed=(l == 1), tail_cb=None)
                    norm_to_hT(A2T, l, b, 24, p1_done=True, act_subs=(0, 1))
                    if l == 0:
                        moe(l, b, t, tail_cb=norm_p1)
                        norm_to_hT(A1T, 1, b, 0, p1_done=True, act_subs=(0, 1))
                    else:
                        moe(l, b, t, tail_cb=final_sub)
        for s_ in range(NSUB):
            for key_ in out_sub[s_].res[0].dsem.values():
                K._wait("sp", {key_: K.cnt[key_]})
        if dbg is not None and dbg_d.res[0].dsem is not None:
            for key_ in dbg_d.res[0].dsem.values():
                K._wait("sp", {key_: K.cnt[key_]})
        assert wpos[0] == len(sched), (wpos, len(sched))
        print("built: inst=%d waits=%d dsems=%d" % (K.ninst, K.nwait, K.ndsem))
    return nc


def make_consts(S):
    c = {}
    c["c_ident"] = np.eye(128, dtype=np.float32)
    P = np.zeros((128, 128), np.float32)
    for d in range(64):
        P[d + 64, d] = -1.0
        P[d, d + 64] = 1.0
    c["c_prot"] = P
    l = np.arange(128)[:, None]
    j = np.arange(128)[None, :]
    mA = ((l // 32 == j // 32) & (l <= j)).astype(np.float32)
    c["c_maskA"] = np.tile(mA, (1, 4))
    gam = 1.0 - 2.0 ** (-5.0 - np.arange(4, dtype=np.float64))
    sc = 128.0 ** -0.5
    intra = np.zeros((128, 4, 128), np.float64)
    qdec = np.zeros((128, 4, 128), np.float64)
    kdec = np.zeros((128, 4, 128), np.float64)
    same = (l // 64 == j // 64)
    for h in range(4):
        intra[:, h, :] = np.where(same, gam[h] ** np.abs(l - j), 0.0) * sc
        qdec[:, h, :] = (gam[h] ** ((np.arange(128) % 64) + 1.0))[None, :] * sc
        kdec[:, h, :] = (gam[h] ** (63.0 - (np.arange(128) % 64)))[:, None]
    c["c_intra"] = intra.reshape(128, 512).astype(np.float32)
    c["c_qdec"] = qdec.reshape(128, 512).astype(np.float32)
    c["c_kdec"] = kdec.reshape(128, 512).astype(np.float32)
    sm = np.ones((128, 512), np.float32)
    sm[:, ::32] = 0.0
    c["c_smask"] = sm
    half = 64
    inv = 10000.0 ** (-np.arange(half, dtype=np.float32) / half)
    ang = np.arange(S, dtype=np.float32)[None, :] * np.concatenate([inv, inv])[:, None].astype(np.float32)
    c["c_cos"] = np.cos(ang).astype(np.float32)
    c["c_sin"] = np.sin(ang).astype(np.float32)
    return c


def make_in_maps(inp, S, ncore=NCORE):
    f = lambda a: np.ascontiguousarray(np.asarray(a, dtype=np.float32))
    consts = make_consts(S)
    shared = dict(consts)
    shared["w_ada"] = f(inp["w_ada"])
    shared["b_adaT"] = f(np.asarray(inp["b_ada"]).reshape(2, 48, 128).transpose(2, 0, 1))
    shared["g1T"] = f(np.asarray(inp["g_norm1"]).reshape(2, 8, 128).transpose(2, 0, 1))
    shared["g2T"] = f(np.asarray(inp["g_norm2"]).reshape(2, 8, 128).transpose(2, 0, 1))
    shared["w_in"] = f(inp["w_in"])
    shared["lbT"] = f(np.asarray(inp["lb_logits"]).reshape(2, 4, 128).transpose(2, 0, 1))
    shared["ghT"] = f(np.asarray(inp["g_hgrn"]).transpose(2, 0, 1))
    shared["grT"] = f(np.asarray(inp["g_ret"]).reshape(2, 4, 2, 128).transpose(3, 0, 1, 2).reshape(128, 2, 8))
    shared["w_branch_a"] = f(inp["w_branch_a"])
    shared["w_branch_b"] = f(inp["w_branch_b"])
    shared["w_out"] = f(inp["w_out"])
    shared["w_routerT"] = f(np.asarray(inp["w_router"]).reshape(8, 128, 16).transpose(1, 0, 2))
    shared["b_router_bc"] = f(np.broadcast_to(np.asarray(inp["b_router"])[None, :], (128, 16)))
    shared["w_exp_gate"] = f(inp["w_exp_gate"])
    shared["w_exp_up"] = f(inp["w_exp_up"])
    shared["w_exp_down"] = f(inp["w_exp_down"])
    shared["g_final_bc"] = f(np.broadcast_to(np.asarray(inp["g_final"])[None, :], (128, D)))
    x = np.asarray(inp["x"])
    c = np.asarray(inp["c"])
    maps = []
    for i in range(ncore):
        m = dict(shared)
        m["x"] = f(x[2 * i:2 * i + 2, :S])
        m["cT"] = f(c[2 * i:2 * i + 2].T.reshape(8, 128, 2).transpose(1, 0, 2))
        maps.append(m)
    return maps


_NC_CACHE = {}


def run(inputs, ncore=NCORE, dbg=None):
    S = int(np.asarray(inputs["x"]).shape[1])
    key = (S, dbg)
    if key not in _NC_CACHE:
        _NC_CACHE[key] = build(S, dbg)
    nc = _NC_CACHE[key]
    maps = make_in_maps(inputs, S, ncore)
    res = run_bass_kernel_spmd(nc, maps, core_ids=list(range(ncore)))
    out = np.concatenate([np.asarray(r["out"]) for r in res.results], axis=0)
    if dbg is not None:
        return out.astype(np.float32), [np.asarray(r["dbg"]) for r in res.results]
    return out.astype(np.float32)


def kernel(**inputs):
    return run(inputs, NCORE)
```

```python
import numpy as np
from contextlib import ExitStack
import concourse.bass as bass
import concourse.mybir as mybir
from concourse.bass_utils import run_bass_kernel_spmd

F32 = mybir.dt.float32
BF16 = mybir.dt.bfloat16
AF = mybir.ActivationFunctionType
ALU = mybir.AluOpType
AX = mybir.AxisListType

D = 1024
TT = 512
NSUB = 4
EPS = 1e-6
NCORE = 8
NSLOT = 4


class Res:
    __slots__ = ("name", "w", "r", "dsem")

    def __init__(self, name):
        self.name = name
        self.w = None
        self.r = {}
        self.dsem = None


class V:
    __slots__ = ("ap", "res")

    def __init__(self, ap, res):
        self.ap = ap
        self.res = res

    def __getitem__(self, idx):
        return V(self.ap[idx], self.res)

    def v(self, ap):
        return V(ap, self.res)

    def re(self, s, **kw):
        return V(self.ap.rearrange(s, **kw), self.res)

    def bc(self, shape):
        return V(self.ap.to_broadcast(shape), self.res)

    def bitcast(self, dt):
        return V(self.ap.bitcast(dt), self.res)


class Sched:
    def __init__(self, nc, st):
        self.nc = nc
        self.st = st
        self.eng = {"pe": nc.tensor, "act": nc.scalar, "dve": nc.vector, "pool": nc.gpsimd, "sp": nc.sync}
        self.sems = {}
        self.cnt = {}
        self.seen = {k: {} for k in self.eng}
        for k in self.eng:
            self.sems[k] = st.enter_context(nc.semaphore("sem_" + k))
            self.cnt[k] = 0
        self.ndsem = 0
        self.nwait = 0
        self.ninst = 0

    def new_dsem(self):
        key = ("d", self.ndsem)
        self.ndsem += 1
        self.sems[key] = self.st.enter_context(self.nc.semaphore("dsem%d" % key[1]))
        self.cnt[key] = 0
        return key

    def _deps(self, reads, writes, skipkey=None):
        deps = {}

        def add(tok):
            if tok is None:
                return
            k, v = tok
            if k == skipkey:
                return
            if deps.get(k, 0) < v:
                deps[k] = v
        for r in reads:
            add(r.w)
        for w in writes:
            add(w.w)
            for k, v in w.r.items():
                add((k, v))
        return deps

    def _wait(self, E, deps):
        seen = self.seen[E]
        for k, v in deps.items():
            if seen.get(k, 0) >= v:
                continue
            self.eng[E].wait_ge(self.sems[k], v)
            seen[k] = v
            self.nwait += 1

    def _mark(self, tok, reads, writes):
        k, v = tok
        for r in reads:
            if r.r.get(k, 0) < v:
                r.r[k] = v
        for w in writes:
            w.w = tok
            w.r = {}

    def op(self, E, fn, reads, writes, inc=True):
        deps = self._deps(reads, writes)
        if E == "pe":
            deps.pop("pe", None)
        self._wait(E, deps)
        inst = fn()
        self.ninst += 1
        if inc:
            self.cnt[E] += 1
            inst.then_inc(self.sems[E], 1)
            tok = (E, self.cnt[E])
        else:
            tok = (E, self.cnt[E] + 1)
        self._mark(tok, reads, writes)
        return inst

    def dma(self, Q, out, in_, **kw):
        reads, writes = in_.res, out.res
        wres = writes[0]
        if wres.dsem is None:
            wres.dsem = {}
        qk = "sw" if Q == "pool" else "hw"
        if qk not in wres.dsem:
            wres.dsem[qk] = self.new_dsem()
        key = wres.dsem[qk]
        self._wait(Q, self._deps(reads, writes, skipkey=key))
        inst = self.eng[Q].dma_start(out=out.ap, in_=in_.ap, **kw)
        self.ninst += 1
        self.cnt[key] += 16
        inst.then_inc(self.sems[key], 16)
        self._mark((key, self.cnt[key]), reads, writes)


def _res(*vs):
    out = []
    for v in vs:
        if isinstance(v, V):
            for r in v.res:
                if r not in out:
                    out.append(r)
    return out


def _ap(v):
    return v.ap if isinstance(v, V) else v


def build(S=4096, dbg=None):
    NT = S // TT
    nc = bass.Bass("TRN2", target_bir_lowering=False)
    st = ExitStack()
    with st:
        K = Sched(nc, st)

        def din(name, shape, dt=F32):
            return V(nc.dram_tensor(name, list(shape), dt, kind="ExternalInput").ap(), [Res(name)])

        x_d = din("x", [2, S, D])
        cT_d = din("cT", [128, 8, 2])
        w_ada_d = din("w_ada", [2, D, 6 * D])
        b_adaT_d = din("b_adaT", [128, 2, 48])
        g1T_d = din("g1T", [128, 2, 8])
        g2T_d = din("g2T", [128, 2, 8])
        w_in_d = din("w_in", [2, D, 7168])
        lbT_d = din("lbT", [128, 2, 4])
        ghT_d = din("ghT", [128, 2, 4])
        grT_d = din("grT", [128, 2, 8])
        wa_d = din("w_branch_a", [2, 512, D])
        wb_d = din("w_branch_b", [2, D, D])
        wo_d = din("w_out", [2, D, D])
        wr_d = din("w_routerT", [128, 8, 16])
        br_d = din("b_router_bc", [128, 16])
        wg_d = din("w_exp_gate", [2, 16, D, 512])
        wu_d = din("w_exp_up", [2, 16, D, 512])
        wd_d = din("w_exp_down", [2, 16, 512, D])
        gfin_d = din("g_final_bc", [128, D])
        ident_d = din("c_ident", [128, 128])
        prot_d = din("c_prot", [128, 128])
        maskA_d = din("c_maskA", [128, 512])
        intra_d = din("c_intra", [128, 512])
        qdec_d = din("c_qdec", [128, 512])
        kdec_d = din("c_kdec", [128, 512])
        smask_d = din("c_smask", [128, 512])
        cos_d = din("c_cos", [128, S])
        sin_d = din("c_sin", [128, S])
        out_ap = nc.dram_tensor("out", [2, S, D], F32, kind="ExternalOutput").ap()
        out_sub = [V(out_ap, [Res("out%d" % s_)]) for s_ in range(NSUB)]
        scr_d = V(nc.dram_tensor("mod_scr", [2, 48, 2, 128], F32, kind="Internal").ap(), [Res("scr")])
        if dbg is not None:
            dbg_d = V(nc.dram_tensor("dbg", list(dbg), F32, kind="ExternalOutput").ap(), [Res("dbg")])

        def sb(name, shape, dt):
            t = st.enter_context(nc.sbuf_tensor("s_" + name, list(shape), dt))
            return V(t[:], [Res(name)])

        xt = [sb("x%d" % s, [128, D], F32) for s in range(NSUB)]
        hT = sb("hT", [128, 8, TT], BF16)
        ring = [sb("ring%d" % i, [128, 4096], BF16) for i in range(NSLOT)]
        BP = [sb("bp%d" % i, [128, 4, TT], BF16) for i in range(11)]
        Fe = sb("Fe", [128, 4, TT], F32)
        FBt = st.enter_context(nc.sbuf_tensor("s_FB", [128, 6, TT], F32))
        FBres = [Res("FB%d" % i) for i in range(6)]
        FBv = [V(FBt[:, i, :], [FBres[i]]) for i in range(6)]
        F1, F2, F3 = FBv[0:2], FBv[2:4], FBv[4:6]
        GB = [V(FBt[:, 2 * i:2 * i + 2, :].bitcast(BF16).rearrange("p a (k c) -> p (a k) c", k=2), [FBres[2 * i], FBres[2 * i + 1]])
              for i in range(3)]
        kinv_s = [sb("kinv_s%d" % i, [128, 512], BF16) for i in range(2)]
        ATa_s = [sb("ATa_s%d" % i, [128, 512], BF16) for i in range(2)]
        krd_s = [sb("krd_s%d" % i, [128, 512], BF16) for i in range(2)]
        ATb_s = [sb("ATb_s%d" % i, [128, 512], BF16) for i in range(2)]
        xb4 = [sb("xbn%d" % i, [128, D], BF16) for i in range(4)]
        xb = xb4
        tmpf = [sb("tmpf%d" % i, [128, TT], F32) for i in range(4)]
        tmpb = [sb("tmpb%d" % i, [128, TT], BF16) for i in range(3)]
        S_a = [[sb("S_a%d_%d" % (l, i), [128, 512], F32) for i in range(2)] for l in range(2)]
        R_b = [sb("R_b%d" % l, [128, 1024], F32) for l in range(2)]
        S_bf = [sb("S_bf%d" % i, [128, 512], BF16) for i in range(4)]
        R_bf = [sb("R_bf%d" % i, [128, 1024], BF16) for i in range(3)]
        stmp = sb("stmp", [128, 512], F32)
        gtb = sb("gtb", [128, D], F32)
        gfin = sb("gfin", [128, D], F32)
        cosS = sb("cosS", [128, TT], F32)
        sinS = sb("sinS", [128, TT], F32)
        ident = sb("ident", [128, 128], BF16)
        prot = sb("prot", [128, 128], BF16)
        ones = sb("ones", [128, 128], BF16)
        maskA = sb("maskA", [128, 512], F32)
        intra = sb("intra", [128, 512], F32)
        qdec = sb("qdec", [128, 512], F32)
        kdec = sb("kdec", [128, 512], F32)
        smask = sb("smask", [128, 512], F32)
        cT = sb("cT", [128, 8, 2], F32)
        ca = sb("ca", [128, 8, 2], F32)
        b_adaT = sb("b_adaT", [128, 2, 48], F32)
        modT = sb("modT", [128, 2, 48, 2], F32)
        modrow = sb("modrow", [96, 128], F32)
        g1T = sb("g1T", [128, 2, 8], F32)
        g2T = sb("g2T", [128, 2, 8], F32)
        A1T = sb("A1T", [128, 2, 2, 8], F32)
        A2T = sb("A2T", [128, 2, 2, 8], F32)
        lbT = sb("lbT", [128, 2, 4], F32)
        oml = sb("oml", [128, 2, 4], F32)
        ghT = sb("ghT", [128, 2, 4], F32)
        grT = sb("grT", [128, 2, 8], F32)
        wr = sb("wr", [128, 8, 16], BF16)
        br = sb("br", [128, 16], F32)
        ss = sb("ss", [128, 8], F32)
        rstd = sb("rstd", [128, 8], F32)
        comb = sb("comb", [128, NSUB, 16], F32)
        rt4 = [sb("rt%d" % i, [128, 64], F32) for i in range(4)]
        rs4 = [sb("rs%d" % i, [128, 16], F32) for i in range(10)]
        rs = rs4

        if dbg is not None:
            tapbuf = sb("tapbuf", [128, 512], F32)
        pst = st.enter_context(nc.psum_tensor("ps", [128, 8, 512], F32))
        psres = [Res("ps%d" % i) for i in range(8)]
        pscur = [0]

        def bank(n=1):
            i = pscur[0]
            if n == 2 and i % 2 == 1:
                i = (i + 1) % 8
            pscur[0] = (i + n) % 8
            if n == 1:
                return V(pst[:, i, :], [psres[i]])
            return V(pst[:, i:i + 2, :], [psres[i], psres[i + 1]])

        def mm(out, lhsT, rhs, start=True, stop=True, last=None, tp=None):
            if last is None:
                last = stop
            kw = {}
            if tp is not None:
                kw["tile_position"] = tp
            K.op("pe", lambda: nc.tensor.matmul(out.ap, lhsT.ap, rhs.ap, start=start, stop=stop, **kw),
                 _res(lhsT, rhs), _res(out), inc=last)

        def tr(out, in_, idv, last=True):
            K.op("pe", lambda: nc.tensor.transpose(out.ap, in_.ap, idv.ap), _res(in_, idv), _res(out), inc=last)

        def act(out, in_, func, scale=None, bias=None, accum=None):
            kw = {}
            if scale is not None:
                kw["scale"] = _ap(scale)
            if bias is not None:
                kw["bias"] = _ap(bias)
            if accum is not None:
                kw["accum_out"] = accum.ap
            K.op("act", lambda: nc.scalar.activation(out=out.ap, in_=in_.ap, func=func, **kw),
                 _res(in_, scale, bias), _res(out, accum))

        def tt(E, out, in0, in1, op):
            K.op(E, lambda: K.eng[E].tensor_tensor(out=out.ap, in0=in0.ap, in1=in1.ap, op=op),
                 _res(in0, in1), _res(out))

        def ts(E, out, in0, s1, op0, s2=None, op1=None):
            if E == "pool" and op1 is None:
                s2, op1 = 0.0, ALU.add

            def f():
                if op1 is None:
                    return K.eng[E].tensor_scalar(out=out.ap, in0=in0.ap, scalar1=_ap(s1), scalar2=None, op0=op0)
                return K.eng[E].tensor_scalar(out=out.ap, in0=in0.ap, scalar1=_ap(s1), scalar2=_ap(s2),
                                              op0=op0, op1=op1)
            K.op(E, f, _res(in0, s1, s2), _res(out))

        def stt(out, in0, scalar, in1, op0, op1):
            K.op("dve", lambda: nc.vector.scalar_tensor_tensor(out=out.ap, in0=in0.ap, scalar=_ap(scalar),
                                                                in1=in1.ap, op0=op0, op1=op1),
                 _res(in0, scalar, in1), _res(out))

        def cp(E, out, in_):
            if E == "act":
                K.op("act", lambda: nc.scalar.copy(out=out.ap, in_=in_.ap), _res(in_), _res(out))
            else:
                K.op(E, lambda: K.eng[E].tensor_copy(out=out.ap, in_=in_.ap), _res(in_), _res(out))

        def memset(E, out, val):
            K.op(E, lambda: K.eng[E].memset(out.ap, val), [], _res(out))

        ident_f = Fe[:, 0, 0:128]
        prot_f = Fe[:, 1, 0:128]
        wr_f = Fe[:, 2, 0:128].re("p (k e) -> p k e", k=8)
        epsb_t = sb("epsb", [128, 1], F32)
        memset("dve", epsb_t, EPS)
        epsb = epsb_t
        for dst, src in ((ident_f, ident_d), (prot_f, prot_d), (maskA, maskA_d), (intra, intra_d), (qdec, qdec_d),
                         (kdec, kdec_d), (smask, smask_d), (cT, cT_d), (b_adaT, b_adaT_d), (g1T, g1T_d),
                         (g2T, g2T_d), (lbT, lbT_d), (ghT, ghT_d), (grT, grT_d), (wr_f, wr_d), (br, br_d),
                         (gfin, gfin_d)):
            K.dma("sp", dst, src)
        cp("dve", ident, ident_f)
        cp("dve", prot, prot_f)
        cp("dve", wr, wr_f)
        memset("dve", ones, 1.0)
        memset("dve", oml[:, 0, :], 1.0)
        tt("dve", rs[0][:, 0:4], lbT[:, 0, :], lbT[:, 1, :], ALU.subtract)
        act(oml[:, 1, :], rs[0][:, 0:4], AF.Sigmoid)
        act(ca, cT, AF.Silu)

        sched = []

        def wslice(src_rows_ap, kb, cols):
            sched.append((src_rows_ap, kb, cols))

        ZC = dict(qa=0, fa=512, ia=1024, oga=1536, qb=2048, kb=2560, vb=3072, ogb=4096, ma=5120, mb=6144)
        for b in range(2):
            for t in range(NT):
                for l in range(2):
                    def wi(c0, l=l):
                        wslice(w_in_d[l][:, c0:c0 + 512].re("(k p) c -> p k c", p=128), 8, 512)
                    for c0 in (ZC["qa"], ZC["fa"], ZC["ia"]):
                        wi(c0)
                    for c0 in (ZC["qb"], ZC["kb"], ZC["vb"], ZC["vb"] + 512):
                        wi(c0)
                    for c0 in (ZC["ma"], ZC["ma"] + 512, ZC["mb"]):
                        wi(c0)
                    for c0 in (ZC["oga"], ZC["ogb"], ZC["ogb"] + 512):
                        wi(c0)
                    wi(ZC["mb"] + 512)
                    wslice(wa_d[l].re("(k p) c -> p k c", p=128), 4, 1024)
                    wslice(wb_d[l][:, 0:512].re("(k p) c -> p k c", p=128), 8, 512)
                    wslice(wb_d[l][:, 512:1024].re("(k p) c -> p k c", p=128), 8, 512)
                    wslice(wo_d[l][:, 0:512].re("(k p) c -> p k c", p=128), 8, 512)
                    wslice(wo_d[l][:, 512:1024].re("(k p) c -> p k c", p=128), 8, 512)
                    wslice(wg_d[l, 0].re("(k p) c -> p k c", p=128), 8, 512)
                    wslice(wu_d[l, 0].re("(k p) c -> p k c", p=128), 8, 512)
                    for e in range(16):
                        if e < 15:
                            wslice(wg_d[l, e + 1].re("(k p) c -> p k c", p=128), 8, 512)
                            wslice(wu_d[l, e + 1].re("(k p) c -> p k c", p=128), 8, 512)
                        wslice(wd_d[l, e].re("(k p) c -> p k c", p=128), 4, 1024)
        wpos = [0, 0]

        NSL = len(sched) // (2 * NT * 2)
        assert NSL * 2 * NT * 2 == len(sched)
        wscr_ap = nc.dram_tensor("w_scr", [2 * NSL, 128, 4096], BF16, kind="Internal").ap()
        scrw_res = [Res("scrw%d" % i) for i in range(NSLOT)]

        def wgetn(n):
            i = wpos[0]
            assert n <= NSLOT
            wpos[0] += n
            while wpos[1] < len(sched) and wpos[1] <= i + NSLOT - 1:
                j = wpos[1]
                src, kb, cols = sched[j]
                tl, k = divmod(j, NSL)
                lyr = tl % 2
                slot = ring[j % NSLOT]
                if tl < 2:
                    K.dma("pool", slot.re("p (k c) -> p k c", k=kb), src)
                    K.dma("sp", V(wscr_ap[lyr * NSL + k], [scrw_res[j % NSLOT]]), slot)
                else:
                    j0 = lyr * NSL + k
                    K.dma("sp", slot, V(wscr_ap[lyr * NSL + k], [scrw_res[j0 % NSLOT]]))
                wpos[1] += 1
            return [ring[j % NSLOT].re("p (k c) -> p k c", k=sched[j][1]) for j in range(i, i + n)]

        def wget():
            return wgetn(1)[0]

        adaring = [ring[i].re("p (k c) -> p k c", k=8) for i in range(NSLOT)]
        ca_bf = sb("ca_bf", [128, 8, 2], BF16)
        cp("dve", ca_bf, ca)
        for l in range(2):
            pb = bank()
            for s in range(12):
                wsl = adaring[s % NSLOT]
                K.dma("pool", wsl, w_ada_d[l][:, s * 512:(s + 1) * 512].re("(k p) c -> p k c", p=128))
                for cbk in range(4):
                    blk = s * 4 + cbk
                    for kb in range(8):
                        mm(pb[:, blk * 2:blk * 2 + 2], wsl[:, kb, cbk * 128:(cbk + 1) * 128], ca_bf[:, kb, :],
                           start=(kb == 0), stop=(kb == 7))
            tt("dve", modT[:, l], pb[:, 0:96].re("p (k b) -> p k b", b=2),
               b_adaT[:, l, :].re("p (k o) -> p k o", o=1).bc([128, 48, 2]), ALU.add)
            for b in range(2):
                stt(A1T[:, l, b, :], modT[:, l, 8:16, b], 1.0, g1T[:, l, :], ALU.add, ALU.mult)
                stt(A2T[:, l, b, :], modT[:, l, 32:40, b], 1.0, g2T[:, l, :], ALU.add, ALU.mult)
            pb2 = bank()
            tr(pb2[:96, :128], modT[:, l].re("p k b -> p (k b)"), ident_f)
            cp("dve", modrow, pb2[:96, :128])
            K.dma("sp", scr_d[l].re("k b p -> (k b) p"), modrow)

        def load_gate(l, b, blk0):
            src = scr_d[l, blk0:blk0 + 8, b, :]
            K.dma("sp", gtb.re("p (k c) -> p k c", k=8), V(src.ap.partition_broadcast(128), src.res))

        def norm_p1(s, phase=None):
            if phase in (None, 0):
                act(xb4[s], xt[s], AF.Square, accum=ss[:, s:s + 1])
            if phase in (None, 1):
                ts("dve", rstd[:, s:s + 1], ss[:, s:s + 1], 1.0 / D, ALU.mult, EPS, ALU.add)
                act(rstd[:, s:s + 1], rstd[:, s:s + 1], AF.Ln)
                act(rstd[:, s:s + 1], rstd[:, s:s + 1], AF.Exp, scale=-0.5)
            if phase in (None, 2):
                ts("pool", xb4[s], xt[s], rstd[:, s:s + 1], ALU.mult)

        def norm_p2(AT, l, b, shblk, s, on_act):
            pb = bank().bitcast(BF16)
            for kb in range(8):
                tr(pb[:, kb * 128:(kb + 1) * 128], xb4[s][:, kb * 128:(kb + 1) * 128], ident, last=(kb == 7))
            if on_act:
                for kb in range(8):
                    act(hT[:, kb, s * 128:(s + 1) * 128], pb[:, kb * 128:(kb + 1) * 128], AF.Identity,
                        scale=AT[:, l, b, kb:kb + 1], bias=modT[:, l, shblk + kb, b:b + 1])
            else:
                for hf in range(2):
                    hv = tmpf[hf].re("p (k j) -> p k j", k=4)
                    tt("dve", hv, pb[:, hf * 512:(hf + 1) * 512].re("p (k j) -> p k j", k=4),
                       AT[:, l, b, 4 * hf:4 * hf + 4].re("p (k o) -> p k o", o=1).bc([128, 4, 128]), ALU.mult)
                    tt("dve", hT[:, 4 * hf:4 * hf + 4, s * 128:(s + 1) * 128], hv,
                       modT[:, l, shblk + 4 * hf:shblk + 4 * hf + 4, b:b + 1].bc([128, 4, 128]), ALU.add)

        def norm_to_hT(AT, l, b, shblk, p1_done=False, act_subs=(0,)):
            if not p1_done:
                for s in range(NSUB):
                    norm_p1(s)
            for s in range(NSUB):
                norm_p2(AT, l, b, shblk, s, s in act_subs)

        def proj_fm(W, nblk, evac):
            for blk in range(nblk):
                pb = bank()
                for kb in range(8):
                    mm(pb, W[:, kb, blk * 128:(blk + 1) * 128], hT[:, kb, :], start=(kb == 0), stop=(kb == 7))
                evac(blk, pb)

        def proj_tm(W, evac):
            for s in range(NSUB):
                pb = bank()
                for kb in range(8):
                    mm(pb, hT[:, kb, s * 128:(s + 1) * 128], W[:, kb, :], start=(kb == 0), stop=(kb == 7))
                evac(s, pb)

        tapidx = [0]
        tapnames = []

        def tap(name, v, cond=True):
            if dbg is None or not cond:
                return
            i = tapidx[0]
            if i >= dbg[0]:
                return
            tapidx[0] += 1
            tapnames.append(name)
            n = v.ap.shape[-1] if len(v.ap.shape) == 2 else None
            if v.ap.dtype != F32:
                cp("dve", tapbuf[:, 0:n], v)
                v = tapbuf[:, 0:n]
            K.dma("sp", dbg_d[i, 0:v.ap.shape[0], 0:n], v)
        build.tapnames = tapnames

        def mixer(l, b, t, prenormed, tail_cb):
            tok0 = t * TT
            T0 = (l == 0 and b == 0 and t == 0)
            K.dma("sp", cosS, cos_d[:, tok0:tok0 + TT])
            K.dma("sp", sinS, sin_d[:, tok0:tok0 + TT])
            if not prenormed:
                norm_to_hT(A1T, l, b, 0)
            qeT, kinvT, va_tm, oaT, qrT, krT, qdT = BP[0:7]
            vb_tm = [BP[7], BP[8]]
            obT = [BP[9], BP[10]]
            qsT = BP[9]
            W = wget()
            proj_fm(W, 4, lambda blk, pb: act(qsT[:, blk, :], pb, AF.Silu))
            W = wget()

            pending = []

            def pump(n):
                for _ in range(n):
                    if not pending:
                        return
                    g = pending[0]
                    try:
                        next(g)
                    except StopIteration:
                        pending.pop(0)

            def chain_f(blk, f1, f2, f3):
                act(f2, f1, AF.Ln, bias=1.0)
                yield
                act(f2, f2, AF.Exp, scale=-1.0)
                ts("dve", f1, f2, oml[:, l, blk:blk + 1], ALU.mult)
                yield
                act(f2, f1, AF.Ln, scale=-1.0, bias=1.0)
                K.op("dve", lambda: nc.vector.tensor_tensor_scan(out=f3.ap, data0=smask.ap, data1=f2.ap,
                                                                  initial=0.0, op0=ALU.mult, op1=ALU.add),
                     _res(smask, f2), _res(f3))
                yield
                act(Fe[:, blk, :], f3, AF.Exp)
                yield
                act(f2, f3, AF.Exp, scale=-1.0)
                tt("dve", kinvT[:, blk, :], f1, f2, ALU.mult)
                tt("dve", qeT[:, blk, :], qsT[:, blk, :], Fe[:, blk, :], ALU.mult)

            def ev_f(blk, pb):
                f1, f2, f3 = F1[blk % 2], F2[blk % 2], F3[blk % 2]
                while len(pending) >= 2:
                    pump(1)
                act(f1, pb, AF.Exp)
                pending.append(chain_f(blk, f1, f2, f3))
                pump(1)
            proj_fm(W, 4, ev_f)
            W = wget()

            def ev_ia(s, pb):
                cp("act", va_tm[:, s, :], pb)
                pump(2)
            proj_tm(W, ev_ia)
            rot_defer = []
            for which in range(2):
                W = wget()

                def rot_rest(blk, xbf, which):
                    pr_ = bank()
                    mm(pr_, prot, xbf)
                    ta, tb = tmpf[2 * (blk % 2)], tmpf[2 * (blk % 2) + 1]
                    tt("dve", ta, xbf, cosS, ALU.mult)
                    tt("dve", tb, pr_, sinS, ALU.mult)
                    dst = qrT if which == 0 else krT
                    tt("pool", dst[:, blk, :], ta, tb, ALU.add)
                    if which == 0:
                        tt("dve", qdT[:, blk, :].re("p (r j) -> p r j", r=4), dst[:, blk, :].re("p (r j) -> p r j", r=4),
                           qdec[:, blk * 128:(blk + 1) * 128].re("p (o j) -> p o j", o=1).bc([128, 4, 128]), ALU.mult)

                def ev_rot(blk, pb, which=which):
                    xbf = tmpb[1 + (blk % 2)]
                    if rot_defer:
                        rot_defer.pop(0)()
                    cp("act", xbf, pb)
                    rot_defer.append(lambda blk=blk, xbf=xbf, which=which: rot_rest(blk, xbf, which))
                    pump(2)
                proj_fm(W, 4, ev_rot)
            for half in range(2):
                W = wget()

                def ev_vb(s, pb, half=half):
                    while rot_defer:
                        rot_defer.pop(0)()
                    cp("act", vb_tm[half][:, s, :], pb)
                    pump(2)
                proj_tm(W, ev_vb)
            pump(1000)
            assert not pending
            tap("qeT_0", qeT[:, 0, :], T0)

            def vbv(s, h, vb, rows=slice(0, 128)):
                c0 = h * 256 + vb * 128
                return vb_tm[c0 // 512][rows, s, (c0 % 512):(c0 % 512) + 128]

            gam64 = [float((1.0 - 2.0 ** (-5.0 - h)) ** 64) for h in range(4)]
            def fill_gen():
                for dst in GB:
                    Wf = wget()
                    for blk in range(4):
                        pf = bank()
                        for kb in range(8):
                            mm(pf, Wf[:, kb, blk * 128:(blk + 1) * 128], hT[:, kb, :], start=(kb == 0), stop=(kb == 7))
                        cp("act", dst[:, blk, :], pf)
                        yield
            fg = fill_gen()

            def fill():
                next(fg, None)
            for s in range(NSUB):
                tk = slice(s * 128, (s + 1) * 128)
                kinv_tm, ATa, krd_tm, ATb = kinv_s[s % 2], ATa_s[s % 2], krd_s[s % 2], ATb_s[s % 2]
                pb = bank().bitcast(BF16)
                for h in range(4):
                    tr(pb[:, h * 128:(h + 1) * 128], kinvT[:, h, tk], ident, last=(h == 3))
                cp("act", kinv_tm, pb[:, 0:512])
                pb = bank()
                for h in range(4):
                    mm(pb[:, h * 128:(h + 1) * 128], kinvT[:, h, tk], qeT[:, h, tk], last=(h == 3))
                tt("dve", ATa, pb, maskA, ALU.mult)
                pb = bank()
                for h in range(4):
                    mm(pb[:, h * 128:(h + 1) * 128], krT[:, h, tk], qrT[:, h, tk], last=(h == 3))
                tt("dve", ATb, pb, intra, ALU.mult)
                pb = bank().bitcast(BF16)
                for h in range(4):
                    tr(pb[:, h * 128:(h + 1) * 128], krT[:, h, tk], ident, last=(h == 3))
                tt("dve", krd_tm, pb[:, 0:512], kdec, ALU.mult)
                fill()
                sbfs = []
                for cc in range(4):
                    c = s * 4 + cc
                    sbf = S_bf[c % 4]
                    Scur, Snxt = S_a[l][c % 2], S_a[l][(c + 1) % 2]
                    cp("act", sbf, Scur)
                    sbfs.append(sbf)
                    pr = slice(32 * cc, 32 * cc + 32)
                    pb = bank()
                    for h in range(4):
                        hs = slice(h * 128, (h + 1) * 128)
                        mm(pb[:, hs], kinv_tm[pr, hs], va_tm[pr, s, hs], last=(h == 3), tp=(32 * cc, 0))
                    tt("dve", stmp, Scur, pb, ALU.add)
                    col = s * 128 + 32 * cc + 31
                    tt("dve", Snxt.re("p (h v) -> p h v", h=4), stmp.re("p (h v) -> p h v", h=4),
                       Fe[:, :, col:col + 1].bc([128, 4, 128]), ALU.mult)
                rbfs = []
                for cc in range(2):
                    c = s * 2 + cc
                    rbf = R_bf[c % 3]
                    cp("act", rbf, R_b[l])
                    rbfs.append(rbf)
                    pr = slice(64 * cc, 64 * cc + 64)
                    pu = bank(2)
                    for h in range(4):
                        c0 = h * 256
                        mm(pu[:, h // 2, (h % 2) * 256:(h % 2) * 256 + 256], krd_tm[pr, h * 128:(h + 1) * 128],
                           vb_tm[c0 // 512][pr, s, (c0 % 512):(c0 % 512) + 256], last=(h == 3), tp=(64 * cc, 0))
                    for h in range(4):
                        stt(R_b[l][:, h * 256:(h + 1) * 256], R_b[l][:, h * 256:(h + 1) * 256], gam64[h],
                            pu[:, h // 2, (h % 2) * 256:(h % 2) * 256 + 256], ALU.mult, ALU.add)
                fill()
                po = bank()
                for h in range(4):
                    hs = slice(h * 128, (h + 1) * 128)
                    mm(po[:, hs], va_tm[:, s, hs], ATa[:, hs], start=True, stop=False)
                    for cc in range(4):
                        o0 = h * 128 + 32 * cc
                        mm(po[:, o0:o0 + 32], sbfs[cc][:, hs], qeT[:, h, s * 128 + 32 * cc:s * 128 + 32 * cc + 32],
                           start=False, stop=(cc == 3), last=(cc == 3 and h == 3))
                act(tmpb[0], po, AF.Square)
                po2 = bank(2)
                for h in range(4):
                    for vb in range(2):
                        o0 = ((h % 2) * 2 + vb) * 128
                        mm(po2[:, h // 2, o0:o0 + 128], vbv(s, h, vb), ATb[:, h * 128:(h + 1) * 128],
                           start=True, stop=False)
                        for cc in range(2):
                            c0 = h * 256 + vb * 128
                            mm(po2[:, h // 2, o0 + 64 * cc:o0 + 64 * cc + 64], rbfs[cc][:, c0:c0 + 128],
                               qdT[:, h, s * 128 + 64 * cc:s * 128 + 64 * cc + 64], start=False, stop=(cc == 1),
                               last=(cc == 1 and vb == 1 and h == 3))
                ob16 = [xb[0][:, 0:512], xb[0][:, 512:1024]]
                sq = [xb[1][:, 0:512], xb[1][:, 512:1024]]
                for k in range(2):
                    cp("act", ob16[k], po2[:, k, :])
                    act(sq[k], po2[:, k, :], AF.Square)
                fill()
                pn = bank()
                mm(pn, ones, tmpb[0])
                p1 = bank()
                p2 = bank()
                for h in range(4):
                    k, hh = h // 2, h % 2
                    for vb in range(2):
                        o0 = (hh * 2 + vb) * 128
                        mm(p1[:, h * 128:(h + 1) * 128], ones, ob16[k][:, o0:o0 + 128], start=(vb == 0),
                           stop=(vb == 1), last=False)
                for h in range(4):
                    k, hh = h // 2, h % 2
                    for vb in range(2):
                        o0 = (hh * 2 + vb) * 128
                        mm(p2[:, h * 128:(h + 1) * 128], ones, sq[k][:, o0:o0 + 128], start=(vb == 0),
                           stop=(vb == 1), last=(vb == 1 and h == 3))
                rs_a, mean, var = tmpf[0], tmpf[1], tmpf[2]
                act(rs_a, pn, AF.Ln, scale=1.0 / 128, bias=epsb)
                act(mean, p1, AF.Copy, scale=1.0 / 256)
                act(var, p1, AF.Square, scale=1.0 / 256)
                stt(var, p2, 1.0 / 256, var, ALU.mult, ALU.subtract)
                act(var, var, AF.Ln, bias=epsb)
                act(rs_a, rs_a, AF.Exp, scale=-0.5)
                act(var, var, AF.Exp, scale=-0.5)
                tt("dve", oaT[:, :, tk], po.re("p (h j) -> p h j", h=4), rs_a.re("p (h j) -> p h j", h=4), ALU.mult)
                for k in range(2):
                    ta = tmpf[3]
                    mv = mean[:, k * 256:(k + 1) * 256].re("p (h o j) -> p h o j", h=2, o=1).bc([128, 2, 2, 128])
                    rv = var[:, k * 256:(k + 1) * 256].re("p (h o j) -> p h o j", h=2, o=1).bc([128, 2, 2, 128])
                    tt("dve", ta.re("p (h v j) -> p h v j", h=2, v=2), po2[:, k, :].re("p (h v j) -> p h v j", h=2, v=2),
                       mv, ALU.subtract)
                    tt("pool", obT[k][:, :, tk].re("p (h v) j -> p h v j", h=2), ta.re("p (h v j) -> p h v j", h=2, v=2),
                       rv, ALU.mult)
            for _ in range(12):
                fill()
            W = wget()

            def ev_oga(blk, pb):
                sg = tmpb[1 + blk % 2]
                act(sg, pb, AF.Silu)
                stt(oaT[:, blk, :], sg, ghT[:, l, blk:blk + 1], oaT[:, blk, :], ALU.mult, ALU.mult)
            proj_fm(W, 4, ev_oga)
            for half in range(2):
                W = wget()

                def ev_ogb(blk, pb, half=half):
                    sg = tmpb[1 + blk % 2]
                    act(sg, pb, AF.Silu)
                    stt(obT[half][:, blk, :], sg, grT[:, l, half * 4 + blk:half * 4 + blk + 1], obT[half][:, blk, :],
                        ALU.mult, ALU.mult)
                proj_fm(W, 4, ev_ogb)
            tap("oaT_0", oaT[:, 0, :], T0)
            tap("oaT_3", oaT[:, 3, :], T0)
            tap("qrT_0", qrT[:, 0, :], T0)
            tap("krT_1", krT[:, 1, :], T0)
            tap("obT_0", obT[0][:, 0, :], T0)
            tap("obT_7", obT[1][:, 3, :], T0)
            yT = [BP[5], BP[6]]
            W = wget()
            proj_fm(W, 4, lambda blk, pb: cp("act", BP[4][:, blk, :], pb))
            graw_a = [GB[0], GB[1]]
            graw_b = [GB[2], BP[4]]
            Wa, Wb0_, Wb1_ = wgetn(3)
            Wb = [Wb0_, Wb1_]
            for cb in range(8):
                pa = bank()
                for vb in range(4):
                    mm(pa, Wa[:, vb, cb * 128:(cb + 1) * 128], oaT[:, vb, :], start=(vb == 0), stop=(vb == 3))
                pb = bank()
                for kb in range(8):
                    mm(pb, Wb[cb // 4][:, kb, (cb % 4) * 128:(cb % 4) * 128 + 128], obT[kb // 4][:, kb % 4, :],
                       start=(kb == 0), stop=(kb == 7))
                act(tmpb[1], graw_a[cb // 4][:, cb % 4, :], AF.Sigmoid)
                act(tmpb[2], graw_b[cb // 4][:, cb % 4, :], AF.Sigmoid)
                tt("dve", tmpf[0], pa, tmpb[1], ALU.mult)
                tt("dve", tmpf[1], pb, tmpb[2], ALU.mult)
                tt("pool", yT[cb // 4][:, cb % 4, :], tmpf[0], tmpf[1], ALU.add)
            tap("yT_0", yT[0][:, 0, :], T0)
            tap("yT_7", yT[1][:, 3, :], T0)
            load_gate(l, b, 16)
            tap("gt1", gtb, T0)
            Wo = wgetn(2)
            for s in range(NSUB):
                for half in range(2):
                    pb = bank()
                    for kb in range(8):
                        mm(pb, yT[kb // 4][:, kb % 4, s * 128:(s + 1) * 128], Wo[half][:, kb, :],
                           start=(kb == 0), stop=(kb == 7))
                    hs = slice(half * 512, (half + 1) * 512)
                    tt("dve", tmpf[2 + half], pb, gtb[:, hs], ALU.mult)
                    tt("pool" if half == 0 else "dve", xt[s][:, hs], xt[s][:, hs], tmpf[2 + half], ALU.add)
                norm_p1(s)

        def moe(l, b, t, tail_cb):
            pb = bank()
            for s in range(NSUB):
                for kb in range(8):
                    mm(pb[:, s * 16:(s + 1) * 16], hT[:, kb, s * 128:(s + 1) * 128], wr[:, kb, :],
                       start=(kb == 0), stop=(kb == 7), last=(kb == 7 and s == NSUB - 1))
            sc_, bi_, sel_, w_ = rt4
            act(sc_, pb[:, 0:64], AF.Sigmoid)
            tt("dve", bi_.re("p (u e) -> p u e", u=4), sc_.re("p (u e) -> p u e", u=4),
               br.re("p (o e) -> p o e", o=1).bc([128, 4, 16]), ALU.add)
            b3 = bi_.re("p (u g e) -> p u g e", u=4, g=4)
            a0, a1, a2, a3 = b3[:, :, :, 0], b3[:, :, :, 1], b3[:, :, :, 2], b3[:, :, :, 3]
            p_, q_, r_, s_, m1, m2, gs, gmx, og, wsum = [v.re("p (u g) -> p u g", u=4) for v in rs4]
            tt("dve", p_, a0, a1, ALU.max)
            tt("dve", q_, a0, a1, ALU.min)
            tt("dve", r_, a2, a3, ALU.max)
            tt("dve", s_, a2, a3, ALU.min)
            tt("dve", m1, p_, r_, ALU.max)
            tt("dve", p_, p_, r_, ALU.min)
            tt("dve", q_, q_, s_, ALU.max)
            tt("dve", m2, p_, q_, ALU.max)
            tt("dve", gs, m1, m2, ALU.add)
            K.op("dve", lambda: nc.vector.tensor_reduce(out=gmx.ap[:, :, 0], in_=gs.ap, axis=AX.X, op=ALU.max),
                 _res(gs), _res(gmx))
            tt("dve", og, gs, gmx[:, :, 0:1].bc([128, 4, 4]), ALU.is_ge)
            sel4 = sel_.re("p (u g e) -> p u g e", u=4, g=4)
            tt("dve", sel4, b3, m2.re("p u (g o) -> p u g o", o=1).bc([128, 4, 4, 4]), ALU.is_ge)
            tt("dve", sel4, sel4, og.re("p u (g o) -> p u g o", o=1).bc([128, 4, 4, 4]), ALU.mult)
            tt("dve", w_, sc_, sel_, ALU.mult)
            K.op("dve", lambda: nc.vector.tensor_reduce(out=wsum.ap[:, :, 0], in_=w_.ap.rearrange("p (u e) -> p u e", u=4),
                                                         axis=AX.X, op=ALU.add),
                 _res(w_), _res(wsum))
            K.op("dve", lambda: nc.vector.reciprocal(out=wsum.ap[:, :, 1], in_=wsum.ap[:, :, 0]),
                 _res(wsum), _res(wsum))
            tt("dve", comb, w_.re("p (u e) -> p u e", u=4), wsum[:, :, 1:2].bc([128, 4, 16]), ALU.mult)
            acc = [BP[s].re("p a c -> p (a c)").bitcast(F32) for s in range(NSUB)]
            def gate_up(e):
                Wg, Wu = wgetn(2)
                heT = BP[4 + e % 2]
                for hb in range(4):
                    pg = bank()
                    for kb in range(8):
                        mm(pg, Wg[:, kb, hb * 128:(hb + 1) * 128], hT[:, kb, :], start=(kb == 0), stop=(kb == 7))
                    pu = bank()
                    for kb in range(8):
                        mm(pu, Wu[:, kb, hb * 128:(hb + 1) * 128], hT[:, kb, :], start=(kb == 0), stop=(kb == 7))
                    sg = tmpb[hb % 3]
                    act(sg, pg, AF.Silu)
                    tt("dve", heT[:, hb, :], sg, pu, ALU.mult)

            def down(e):
                Wd = wget()
                heT = BP[4 + e % 2]
                for s in range(NSUB):
                    for half in range(2):
                        pb = bank()
                        for hb in range(4):
                            mm(pb, heT[:, hb, s * 128:(s + 1) * 128], Wd[:, hb, half * 512:(half + 1) * 512],
                               start=(hb == 0), stop=(hb == 3))
                        hs = slice(half * 512, (half + 1) * 512)
                        if e == 0:
                            ts("dve", acc[s][:, hs], pb, comb[:, s, e:e + 1], ALU.mult)
                        else:
                            stt(acc[s][:, hs], pb, comb[:, s, e:e + 1], acc[s][:, hs], ALU.mult, ALU.add)

            gate_up(0)
            for e in range(16):
                if e < 15:
                    gate_up(e + 1)
                down(e)
            load_gate(l, b, 40)
            for s in range(NSUB):
                tt("dve", acc[s], acc[s], gtb, ALU.mult)
                tt("dve", xt[s], xt[s], acc[s], ALU.add)
                tail_cb(s, 0)
            for s in range(NSUB):
                tail_cb(s, 1)
            for s in range(NSUB):
                tail_cb(s, 2)

        for b in range(2):
            for l in range(2):
                memset("pool", S_a[l][0], 0.0)
                memset("pool", R_b[l], 0.0)
            for t in range(NT):
                for s in range(NSUB):
                    K.dma("sp", xt[s], x_d[b, t * TT + s * 128:t * TT + (s + 1) * 128, :])
                def final_sub(s, phase, b=b, t=t):
                    if phase == 0:
                        act(xb4[s], xt[s], AF.Square, accum=ss[:, 4 + s:5 + s])
                    elif phase == 1:
                        ts("dve", rstd[:, 4 + s:5 + s], ss[:, 4 + s:5 + s], 1.0 / D, ALU.mult, EPS, ALU.add)
                        act(rstd[:, 4 + s:5 + s], rstd[:, 4 + s:5 + s], AF.Ln)
                        act(rstd[:, 4 + s:5 + s], rstd[:, 4 + s:5 + s], AF.Exp, scale=-0.5)
                    else:
                        stt(xt[s], xt[s], rstd[:, 4 + s:5 + s], gfin, ALU.mult, ALU.mult)
                        K.dma("sp", out_sub[s][b, t * TT + s * 128:t * TT + (s + 1) * 128, :], xt[s])
                for l in range(2):
                    mixer(l, b, t, prenormed=(l == 1), tail_cb=None)
                    norm_to_hT(A2T, l, b, 24, p1_done=True, act_subs=(0, 1))
                    if l == 0:
                        moe(l, b, t, tail_cb=norm_p1)
                        norm_to_hT(A1T, 1, b, 0, p1_done=True, act_subs=(0, 1))
                    else:
                        moe(l, b, t, tail_cb=final_sub)
        for s_ in range(NSUB):
            for key_ in out_sub[s_].res[0].dsem.values():
                K._wait("sp", {key_: K.cnt[key_]})
        if dbg is not None and dbg_d.res[0].dsem is not None:
            for key_ in dbg_d.res[0].dsem.values():
                K._wait("sp", {key_: K.cnt[key_]})
        assert wpos[0] == len(sched), (wpos, len(sched))
        print("built: inst=%d waits=%d dsems=%d" % (K.ninst, K.nwait, K.ndsem))
    return nc


def make_consts(S):
    c = {}
    c["c_ident"] = np.eye(128, dtype=np.float32)
    P = np.zeros((128, 128), np.float32)
    for d in range(64):
        P[d + 64, d] = -1.0
        P[d, d + 64] = 1.0
    c["c_prot"] = P
    l = np.arange(128)[:, None]
    j = np.arange(128)[None, :]
    mA = ((l // 32 == j // 32) & (l <= j)).astype(np.float32)
    c["c_maskA"] = np.tile(mA, (1, 4))
    gam = 1.0 - 2.0 ** (-5.0 - np.arange(4, dtype=np.float64))
    sc = 128.0 ** -0.5
    intra = np.zeros((128, 4, 128), np.float64)
    qdec = np.zeros((128, 4, 128), np.float64)
    kdec = np.zeros((128, 4, 128), np.float64)
    same = (l // 64 == j // 64)
    for h in range(4):
        intra[:, h, :] = np.where(same, gam[h] ** np.abs(l - j), 0.0) * sc
        qdec[:, h, :] = (gam[h] ** ((np.arange(128) % 64) + 1.0))[None, :] * sc
        kdec[:, h, :] = (gam[h] ** (63.0 - (np.arange(128) % 64)))[:, None]
    c["c_intra"] = intra.reshape(128, 512).astype(np.float32)
    c["c_qdec"] = qdec.reshape(128, 512).astype(np.float32)
    c["c_kdec"] = kdec.reshape(128, 512).astype(np.float32)
    sm = np.ones((128, 512), np.float32)
    sm[:, ::32] = 0.0
    c["c_smask"] = sm
    half = 64
    inv = 10000.0 ** (-np.arange(half, dtype=np.float32) / half)
    ang = np.arange(S, dtype=np.float32)[None, :] * np.concatenate([inv, inv])[:, None].astype(np.float32)
    c["c_cos"] = np.cos(ang).astype(np.float32)
    c["c_sin"] = np.sin(ang).astype(np.float32)
    return c


def make_in_maps(inp, S, ncore=NCORE):
    f = lambda a: np.ascontiguousarray(np.asarray(a, dtype=np.float32))
    consts = make_consts(S)
    shared = dict(consts)
    shared["w_ada"] = f(inp["w_ada"])
    shared["b_adaT"] = f(np.asarray(inp["b_ada"]).reshape(2, 48, 128).transpose(2, 0, 1))
    shared["g1T"] = f(np.asarray(inp["g_norm1"]).reshape(2, 8, 128).transpose(2, 0, 1))
    shared["g2T"] = f(np.asarray(inp["g_norm2"]).reshape(2, 8, 128).transpose(2, 0, 1))
    shared["w_in"] = f(inp["w_in"])
    shared["lbT"] = f(np.asarray(inp["lb_logits"]).reshape(2, 4, 128).transpose(2, 0, 1))
    shared["ghT"] = f(np.asarray(inp["g_hgrn"]).transpose(2, 0, 1))
    shared["grT"] = f(np.asarray(inp["g_ret"]).reshape(2, 4, 2, 128).transpose(3, 0, 1, 2).reshape(128, 2, 8))
    shared["w_branch_a"] = f(inp["w_branch_a"])
    shared["w_branch_b"] = f(inp["w_branch_b"])
    shared["w_out"] = f(inp["w_out"])
    shared["w_routerT"] = f(np.asarray(inp["w_router"]).reshape(8, 128, 16).transpose(1, 0, 2))
    shared["b_router_bc"] = f(np.broadcast_to(np.asarray(inp["b_router"])[None, :], (128, 16)))
    shared["w_exp_gate"] = f(inp["w_exp_gate"])
    shared["w_exp_up"] = f(inp["w_exp_up"])
    shared["w_exp_down"] = f(inp["w_exp_down"])
    shared["g_final_bc"] = f(np.broadcast_to(np.asarray(inp["g_final"])[None, :], (128, D)))
    x = np.asarray(inp["x"])
    c = np.asarray(inp["c"])
    maps = []
    for i in range(ncore):
        m = dict(shared)
        m["x"] = f(x[2 * i:2 * i + 2, :S])
        m["cT"] = f(c[2 * i:2 * i + 2].T.reshape(8, 128, 2).transpose(1, 0, 2))
        maps.append(m)
    return maps


_NC_CACHE = {}


def run(inputs, ncore=NCORE, dbg=None):
    S = int(np.asarray(inputs["x"]).shape[1])
    key = (S, dbg)
    if key not in _NC_CACHE:
        _NC_CACHE[key] = build(S, dbg)
    nc = _NC_CACHE[key]
    maps = make_in_maps(inputs, S, ncore)
    res = run_bass_kernel_spmd(nc, maps, core_ids=list(range(ncore)))
    out = np.concatenate([np.asarray(r["out"]) for r in res.results], axis=0)
    if dbg is not None:
        return out.astype(np.float32), [np.asarray(r["dbg"]) for r in res.results]
    return out.astype(np.float32)


def kernel(**inputs):
    return run(inputs, NCORE)
```
